# Optimizing a Trainium2 kernel written in Bass

```python
import math
import jax, jax.numpy as jnp
from jax import lax
import numpy as np


D_MODEL = 1024
BATCH = 16
SEQ = 4096
DEPTH = 4

CONV_DIM = 512
CONV_WIDTH = 3
NSA_HEADS = 8
NSA_KV_GROUPS = 2
NSA_HPG = NSA_HEADS // NSA_KV_GROUPS
NSA_HEAD_DIM = 64
NSA_DIM = NSA_HEADS * NSA_HEAD_DIM
CMP_LEN = 32
CMP_STRIDE = 16
CMP_HIDDEN = 256
SLC_LEN = 64
SLC_TOPN = 8
SLC_FORCE_BONUS = 1e6
WINDOW = 512
NSA_QBLOCK = 64
GDN_HEADS = 4
GDN_HEAD_DIM = 128
GDN_DIM = GDN_HEADS * GDN_HEAD_DIM
GDN_CONV = 4
GDN_CHUNK = 64
N_BRANCH = 3
BRANCH_DIM = 512
IN_SPLITS = (3 * CONV_DIM, NSA_DIM, 6 * NSA_KV_GROUPS * NSA_HEAD_DIM, 3 * NSA_HEADS, 3 * GDN_DIM, GDN_DIM, GDN_HEADS, GDN_HEADS)
D_IN = 3 * CONV_DIM + NSA_DIM + 6 * NSA_KV_GROUPS * NSA_HEAD_DIM + 3 * NSA_HEADS + 4 * GDN_DIM + 2 * GDN_HEADS
MOE_GROUPS = 4
EXPERTS_PER_GROUP = 8
N_EXPERTS = MOE_GROUPS * EXPERTS_PER_GROUP
TOPK_IN_GROUP = 2
EXPERT_FF = 512
MOE_BLOCK = 256
RMS_EPS = 1e-6
NEG_INF = -1e30

kernel_name = 'hybrid_conv_nsa_gdn_hmoe'


def split_last(u, sizes):
    out, start = [], 0
    for n in sizes:
        out.append(u[..., start:start + n])
        start += n
    return out


def rms_norm(x, gain):
    xf = x.astype(jnp.float32)
    y = xf * lax.rsqrt(jnp.mean(xf * xf, axis=-1, keepdims=True) + RMS_EPS)
    return (y * gain.astype(jnp.float32)).astype(x.dtype)


def l2_norm(x):
    return x * lax.rsqrt(jnp.sum(x * x, axis=-1, keepdims=True) + RMS_EPS)


def causal_depthwise_conv(x, w):
    k, c = w.shape
    return lax.conv_general_dilated(x, w[:, None, :].astype(x.dtype), window_strides=(1,), padding=[(k - 1, 0)], dimension_numbers=('NWC', 'WIO', 'NWC'), feature_group_count=c)


def alibi_slopes(n):
    return 2.0 ** (-8.0 * jnp.arange(1, n + 1, dtype=jnp.float32) / n)


def short_conv_mixer(u_a, conv_w):
    b_gate, c_gate, x_in = split_last(u_a, (CONV_DIM, CONV_DIM, CONV_DIM))
    return b_gate * causal_depthwise_conv(c_gate * x_in, conv_w)


def nsa_compress(kv, pe, w1, w2):
    b, s, g, d = kv.shape
    n_cmp = (s - CMP_LEN) // CMP_STRIDE + 1
    idx = jnp.arange(n_cmp)[:, None] * CMP_STRIDE + jnp.arange(CMP_LEN)[None, :]
    blocks = kv[:, idx] + pe[:, None, :]
    flat = jnp.moveaxis(blocks, 3, 2).reshape(b, n_cmp, g, CMP_LEN * d)
    return jax.nn.gelu(flat @ w1) @ w2


def nsa_mixer(u_q, u_kv, u_g, qk_gain, cmp_pe, cmp_w1, cmp_w2):
    b, s, _ = u_q.shape
    g, hpg, d, qbl = NSA_KV_GROUPS, NSA_HPG, NSA_HEAD_DIM, NSA_QBLOCK
    f32 = jnp.float32
    scale = d ** -0.5
    q = rms_norm(u_q.reshape(b, s, g, hpg, d), qk_gain[0])
    kv = u_kv.reshape(b, s, 6, g, d)
    k_c = rms_norm(nsa_compress(kv[:, :, 0], cmp_pe[0], cmp_w1[0], cmp_w2[0]), qk_gain[1])
    v_c = nsa_compress(kv[:, :, 1], cmp_pe[1], cmp_w1[1], cmp_w2[1])
    n_cmp = k_c.shape[1]
    n_slc = s // SLC_LEN
    top_n = min(SLC_TOPN, n_slc)
    k_s = rms_norm(kv[:, :, 2], qk_gain[2]).reshape(b, n_slc, SLC_LEN, g, d).transpose(0, 3, 1, 2, 4)
    v_s = kv[:, :, 3].reshape(b, n_slc, SLC_LEN, g, d).transpose(0, 3, 1, 2, 4)
    pad = ((0, 0), (WINDOW, 0), (0, 0), (0, 0))
    k_w = jnp.pad(rms_norm(kv[:, :, 4], qk_gain[3]), pad)
    v_w = jnp.pad(kv[:, :, 5], pad)
    gates = jax.nn.sigmoid(u_g.astype(f32)).reshape(b, s, 3, g, hpg)
    slopes = alibi_slopes(NSA_HEADS).reshape(g, hpg)
    cmp_lo = jnp.arange(n_cmp) * CMP_STRIDE
    cmp_end = cmp_lo + CMP_LEN - 1
    slc_lo = jnp.arange(n_slc) * SLC_LEN
    overlap = ((cmp_lo[:, None] < slc_lo[None, :] + SLC_LEN) & (cmp_lo[:, None] + CMP_LEN > slc_lo[None, :])).astype(f32)
    n_qb = s // qbl
    q_blocks = jnp.moveaxis(q.reshape(b, n_qb, qbl, g, hpg, d), 1, 0)
    g_blocks = jnp.moveaxis(gates.reshape(b, n_qb, qbl, 3, g, hpg), 1, 0)
    bi = jnp.arange(b)[:, None, None, None]
    gi = jnp.arange(g)[None, :, None, None]
    j_idx = jnp.arange(n_slc)

    def query_block(args):
        qb, gb, blk = args
        q0 = blk * qbl
        t = q0 + jnp.arange(qbl)
        dist_c = (t[:, None] - cmp_end[None, :]).astype(f32)
        ok_c = dist_c >= 0
        s_c = jnp.einsum('bqghd,bcgd->bghqc', qb, k_c).astype(f32) * scale
        s_c = jnp.where(ok_c, s_c - slopes[:, :, None, None] * dist_c, NEG_INF)
        p_c = jax.nn.softmax(s_c, axis=-1) * jnp.any(ok_c, axis=-1)[:, None].astype(f32)
        o_c = jnp.einsum('bghqc,bcgd->bqghd', p_c.astype(v_c.dtype), v_c)
        imp = jnp.einsum('bghqc,cj->bgqj', p_c, overlap)
        jt = (t // SLC_LEN)[:, None]
        forced = (j_idx[None, :] == 0) | (j_idx[None, :] == jt) | (j_idx[None, :] == jt - 1)
        score = jnp.where(j_idx[None, :] <= jt, imp + jnp.where(forced, SLC_FORCE_BONUS, 0.0), NEG_INF)
        _, sel = lax.top_k(score, top_n)
        k_sel = k_s[bi, gi, sel].reshape(b, g, qbl, top_n * SLC_LEN, d)
        v_sel = v_s[bi, gi, sel].reshape(b, g, qbl, top_n * SLC_LEN, d)
        pos_s = (sel[..., None] * SLC_LEN + jnp.arange(SLC_LEN)).reshape(b, g, qbl, top_n * SLC_LEN)
        dist_s = (t[None, None, :, None] - pos_s).astype(f32)[:, :, None]
        s_s = jnp.einsum('bqghd,bgqkd->bghqk', qb, k_sel).astype(f32) * scale
        s_s = jnp.where(dist_s >= 0, s_s - slopes[None, :, :, None, None] * dist_s, NEG_INF)
        p_s = jax.nn.softmax(s_s, axis=-1)
        o_s = jnp.einsum('bghqk,bgqkd->bqghd', p_s.astype(v_sel.dtype), v_sel)
        k_wb = lax.dynamic_slice_in_dim(k_w, q0, WINDOW + qbl, axis=1)
        v_wb = lax.dynamic_slice_in_dim(v_w, q0, WINDOW + qbl, axis=1)
        pos_w = q0 - WINDOW + jnp.arange(WINDOW + qbl)
        dist_w = t[:, None] - pos_w[None, :]
        ok_w = (dist_w >= 0) & (dist_w < WINDOW) & (pos_w[None, :] >= 0)
        s_w = jnp.einsum('bqghd,bkgd->bghqk', qb, k_wb).astype(f32) * scale
        s_w = jnp.where(ok_w, s_w - slopes[:, :, None, None] * dist_w.astype(f32), NEG_INF)
        p_w = jax.nn.softmax(s_w, axis=-1)
        o_w = jnp.einsum('bghqk,bkgd->bqghd', p_w.astype(v_wb.dtype), v_wb)
        o = gb[:, :, 0, :, :, None] * o_c + gb[:, :, 1, :, :, None] * o_s + gb[:, :, 2, :, :, None] * o_w
        return o.reshape(b, qbl, NSA_DIM).astype(qb.dtype)

    out = lax.map(query_block, (q_blocks, g_blocks, jnp.arange(n_qb)))
    return jnp.moveaxis(out, 0, 1).reshape(b, s, NSA_DIM)


def chunk_gated_delta_rule(q, k, v, g, beta):
    b, s, h, dk = q.shape
    dv = v.shape[-1]
    c = GDN_CHUNK
    n = s // c

    def to_chunks(a):
        return jnp.moveaxis(a.reshape((b, n, c, h) + a.shape[3:]), 3, 1)

    q, k, v, beta = to_chunks(q), to_chunks(k), to_chunks(v), to_chunks(beta)
    gc = jnp.cumsum(to_chunks(g), axis=-1)
    tri = jnp.tril(jnp.ones((c, c), dtype=bool))
    decay = jnp.where(tri, jnp.exp(jnp.where(tri, gc[..., :, None] - gc[..., None, :], 0.0)), 0.0)
    k_beta = k * beta[..., None]
    m = jnp.einsum('bhncd,bhnsd->bhncs', k_beta, k) * decay
    t_inv = lax.linalg.triangular_solve(m, jnp.broadcast_to(jnp.eye(c, dtype=m.dtype), m.shape), left_side=True, lower=True, unit_diagonal=True)
    u = t_inv @ (v * beta[..., None])
    w = t_inv @ (k_beta * jnp.exp(gc)[..., None])
    attn = jnp.einsum('bhncd,bhnsd->bhncs', q, k) * decay
    q_dec = q * jnp.exp(gc)[..., None]
    g_last = gc[..., -1:]
    k_dec = k * jnp.exp(g_last - gc)[..., None]
    chunk_decay = jnp.exp(g_last[..., 0])

    def step(state, xs):
        q_i, k_i, u_i, w_i, a_i, d_i = xs
        v_new = u_i - jnp.einsum('bhcd,bhde->bhce', w_i, state)
        o_i = jnp.einsum('bhcd,bhde->bhce', q_i, state) + jnp.einsum('bhcs,bhse->bhce', a_i, v_new)
        state = state * d_i[..., None, None] + jnp.einsum('bhcd,bhce->bhde', k_i, v_new)
        return state, o_i

    xs = tuple(jnp.moveaxis(a, 2, 0) for a in (q_dec, k_dec, u, w, attn, chunk_decay))
    _, o = lax.scan(step, jnp.zeros((b, h, dk, dv), q.dtype), xs)
    return o.transpose(1, 0, 3, 2, 4).reshape(b, s, h, dv)


def gated_deltanet_mixer(u_qkv, u_z, u_b, u_a, conv_w, a_log, dt_bias, out_gain):
    b, s, _ = u_qkv.shape
    f32 = jnp.float32
    qkv = jax.nn.silu(causal_depthwise_conv(u_qkv, conv_w)).astype(f32).reshape(b, s, 3, GDN_HEADS, GDN_HEAD_DIM)
    q = l2_norm(qkv[:, :, 0]) * GDN_HEAD_DIM ** -0.5
    k = l2_norm(qkv[:, :, 1])
    v = qkv[:, :, 2]
    beta = jax.nn.sigmoid(u_b.astype(f32))
    g = -jnp.exp(a_log.astype(f32)) * jax.nn.softplus(u_a.astype(f32) + dt_bias.astype(f32))
    o = chunk_gated_delta_rule(q, k, v, g, beta)
    o = rms_norm(o, out_gain) * jax.nn.silu(u_z.astype(f32).reshape(b, s, GDN_HEADS, GDN_HEAD_DIM))
    return o.reshape(b, s, GDN_DIM).astype(u_qkv.dtype)


def hybrid_mixer(h, w_in, conv_a_w, nsa_qk_gain, cmp_pe, cmp_w1, cmp_w2, gdn_conv_w, gdn_a_log, gdn_dt_bias, gdn_out_gain, w_branch, w_gate, b_gate, w_out):
    b, s, dm = h.shape
    u = h @ w_in
    u_a, u_nq, u_nkv, u_ng, u_gqkv, u_gz, u_gb, u_ga = split_last(u, IN_SPLITS)
    y_a = short_conv_mixer(u_a, conv_a_w)
    y_n = nsa_mixer(u_nq, u_nkv, u_ng, nsa_qk_gain, cmp_pe, cmp_w1, cmp_w2)
    y_g = gated_deltanet_mixer(u_gqkv, u_gz, u_gb, u_ga, gdn_conv_w, gdn_a_log, gdn_dt_bias, gdn_out_gain)
    branches = jnp.einsum('bsrc,rcd->bsrd', jnp.stack([y_a, y_n, y_g], axis=2), w_branch)
    gates = jax.nn.sigmoid((h @ w_gate + b_gate).astype(jnp.float32)).reshape(b, s, N_BRANCH, dm)
    merged = jnp.sum(gates * branches, axis=2).astype(h.dtype)
    return merged @ w_out


def hier_moe(h, w_rg, b_rg, w_re, b_re, w_eg, w_eu, w_ed):
    b, s, dm = h.shape
    n_tok = b * s
    n_asg = n_tok * TOPK_IN_GROUP
    xt = h.reshape(n_tok, dm)
    lg = (xt @ w_rg + b_rg).astype(jnp.float32)
    grp = jnp.argmax(lg, axis=-1)
    p_grp = jnp.take_along_axis(jax.nn.softmax(lg, axis=-1), grp[:, None], axis=-1)
    le = (xt @ w_re + b_re).astype(jnp.float32).reshape(n_tok, MOE_GROUPS, EXPERTS_PER_GROUP)
    le = jnp.take_along_axis(le, grp[:, None, None], axis=1)[:, 0]
    top_p, top_i = lax.top_k(jax.nn.softmax(le, axis=-1), TOPK_IN_GROUP)
    gate = top_p / jnp.sum(top_p, axis=-1, keepdims=True) * p_grp
    expert = (grp[:, None] * EXPERTS_PER_GROUP + top_i).reshape(n_asg)
    token = jnp.arange(n_asg) // TOPK_IN_GROUP
    order = jnp.argsort(expert)
    e_s, tok_s, gate_s = expert[order], token[order], gate.reshape(n_asg)[order]
    counts = jnp.bincount(expert, length=N_EXPERTS)
    n_blk = (counts + MOE_BLOCK - 1) // MOE_BLOCK
    start = jnp.cumsum(counts) - counts
    pad_start = (jnp.cumsum(n_blk) - n_blk) * MOE_BLOCK
    dest = pad_start[e_s] + jnp.arange(n_asg) - start[e_s]
    total_blk = n_asg // MOE_BLOCK + N_EXPERTS
    buf = jnp.zeros((total_blk * MOE_BLOCK, dm), h.dtype).at[dest].set(xt[tok_s])
    blk_expert = jnp.repeat(jnp.arange(N_EXPERTS), n_blk, total_repeat_length=total_blk)

    def expert_block(args):
        xb, e = args
        return (jax.nn.silu(xb @ w_eg[e]) * (xb @ w_eu[e])) @ w_ed[e]

    yb = lax.map(expert_block, (buf.reshape(total_blk, MOE_BLOCK, dm), blk_expert))
    y = yb.reshape(total_blk * MOE_BLOCK, dm)[dest] * gate_s[:, None].astype(h.dtype)
    return jax.ops.segment_sum(y, tok_s, num_segments=n_tok).reshape(b, s, dm)


def setup_inputs(seed: int = 0) -> dict:
    key = jax.random.key(seed)
    ks = jax.random.split(key, 24)
    f32 = jnp.float32

    def nrm(k, shape, scale):
        return jax.random.normal(k, shape, f32) * scale

    out_scale = (2 * DEPTH) ** -0.5
    dt = jnp.exp(jax.random.uniform(ks[10], (DEPTH, GDN_HEADS), f32, math.log(1e-3), math.log(1e-1)))
    return {
        'x': nrm(ks[0], (BATCH, SEQ, D_MODEL), 1.0),
        'norm_mix': 1.0 + nrm(ks[1], (DEPTH, D_MODEL), 0.02),
        'w_in': nrm(ks[2], (DEPTH, D_MODEL, D_IN), D_MODEL ** -0.5),
        'conv_a_w': nrm(ks[3], (DEPTH, CONV_WIDTH, CONV_DIM), CONV_WIDTH ** -0.5),
        'nsa_qk_gain': 1.0 + nrm(ks[4], (DEPTH, 4, NSA_HEAD_DIM), 0.02),
        'cmp_pe': nrm(ks[5], (DEPTH, 2, CMP_LEN, NSA_HEAD_DIM), 0.02),
        'cmp_w1': nrm(ks[6], (DEPTH, 2, CMP_LEN * NSA_HEAD_DIM, CMP_HIDDEN), (CMP_LEN * NSA_HEAD_DIM) ** -0.5),
        'cmp_w2': nrm(ks[7], (DEPTH, 2, CMP_HIDDEN, NSA_HEAD_DIM), CMP_HIDDEN ** -0.5),
        'gdn_conv_w': nrm(ks[8], (DEPTH, GDN_CONV, 3 * GDN_DIM), GDN_CONV ** -0.5),
        'gdn_a_log': jnp.log(jax.random.uniform(ks[9], (DEPTH, GDN_HEADS), f32, 1.0, 16.0)),
        'gdn_dt_bias': dt + jnp.log(-jnp.expm1(-dt)),
        'gdn_out_gain': 1.0 + nrm(ks[11], (DEPTH, GDN_HEAD_DIM), 0.02),
        'w_branch': nrm(ks[12], (DEPTH, N_BRANCH, BRANCH_DIM, D_MODEL), BRANCH_DIM ** -0.5),
        'w_gate': nrm(ks[13], (DEPTH, D_MODEL, N_BRANCH * D_MODEL), D_MODEL ** -0.5),
        'b_gate': nrm(ks[14], (DEPTH, N_BRANCH * D_MODEL), 0.01),
        'w_out': nrm(ks[15], (DEPTH, D_MODEL, D_MODEL), D_MODEL ** -0.5 * out_scale),
        'norm_ffn': 1.0 + nrm(ks[16], (DEPTH, D_MODEL), 0.02),
        'w_router_group': nrm(ks[17], (DEPTH, D_MODEL, MOE_GROUPS), D_MODEL ** -0.5),
        'b_router_group': nrm(ks[18], (DEPTH, MOE_GROUPS), 0.01),
        'w_router_expert': nrm(ks[19], (DEPTH, D_MODEL, N_EXPERTS), D_MODEL ** -0.5),
        'b_router_expert': nrm(ks[20], (DEPTH, N_EXPERTS), 0.01),
        'w_expert_gate': nrm(ks[21], (DEPTH, N_EXPERTS, D_MODEL, EXPERT_FF), D_MODEL ** -0.5),
        'w_expert_up': nrm(ks[22], (DEPTH, N_EXPERTS, D_MODEL, EXPERT_FF), D_MODEL ** -0.5),
        'w_expert_down': nrm(ks[23], (DEPTH, N_EXPERTS, EXPERT_FF, D_MODEL), EXPERT_FF ** -0.5 * out_scale),
    }


def reference(x, norm_mix, w_in, conv_a_w, nsa_qk_gain, cmp_pe, cmp_w1, cmp_w2, gdn_conv_w, gdn_a_log, gdn_dt_bias, gdn_out_gain, w_branch, w_gate, b_gate, w_out, norm_ffn, w_router_group, b_router_group, w_router_expert, b_router_expert, w_expert_gate, w_expert_up, w_expert_down):
    for l in range(DEPTH):
        h = rms_norm(x, norm_mix[l])
        x = x + hybrid_mixer(h, w_in[l], conv_a_w[l], nsa_qk_gain[l], cmp_pe[l], cmp_w1[l], cmp_w2[l], gdn_conv_w[l], gdn_a_log[l], gdn_dt_bias[l], gdn_out_gain[l], w_branch[l], w_gate[l], b_gate[l], w_out[l])
        h = rms_norm(x, norm_ffn[l])
        x = x + hier_moe(h, w_router_group[l], b_router_group[l], w_router_expert[l], b_router_expert[l], w_expert_gate[l], w_expert_up[l], w_expert_down[l])
    return x
```

```python
import contextlib
import numpy as np
import concourse.bass as bass
import concourse.mybir as mybir

F32 = mybir.dt.float32
BF16 = mybir.dt.bfloat16
AF = mybir.ActivationFunctionType
ALU = mybir.AluOpType
AX = mybir.AxisListType

EPOCH = 30000
NDMA_SLOTS = 6
SB_BASE = 16512
SB_TOP = 229344
ENGS = ["pe", "act", "dve", "pool", "sp"]
DMAQ = ["sp", "pool"]
DTSIZE = {F32: 4, BF16: 2}


class Prog:
    def __init__(self, same_engine_sync=True):
        self.nc = bass.Bass("TRN2", target_bir_lowering=False)
        self.ops = []
        self.same_engine_sync = same_engine_sync
        self.sb_off = SB_BASE
        self.sb_peak = SB_BASE
        self.uid = 0
        self.banks = [self.nc.alloc_psum_tensor(f"bank{i}", [128, 512], F32) for i in range(8)]

    def dram(self, name, shape, dt, kind="Internal"):
        return self.nc.dram_tensor(name, list(shape), dt, kind=kind)

    def sb(self, name, shape, dt):
        n = 1
        for s in shape[1:]:
            n *= s
        nbytes = (n * DTSIZE[dt] + 31) // 32 * 32
        off = self.sb_off
        assert off + nbytes <= SB_TOP, f"SBUF overflow allocating {name}: {off}+{nbytes}"
        self.uid += 1
        t = self.nc.alloc_sbuf_tensor_at(f"{name}_{self.uid}", list(shape), dt, offset=off)
        self.sb_off = off + nbytes
        self.sb_peak = max(self.sb_peak, self.sb_off)
        return t

    def mark(self):
        return self.sb_off

    def release(self, mark):
        self.fence()
        self.sb_off = mark

    def fence(self):
        self.ops.append(("*", None, [], [], False))

    @staticmethod
    def _key(x):
        if isinstance(x, str):
            return x
        if isinstance(x, tuple):
            return Prog._key(x[0]) + "#" + str(x[1])
        t = getattr(x, "tensor", None)
        if t is not None:
            return t.name
        return x.name

    def op(self, eng, fn, r=(), w=()):
        rk = [self._key(k) for k in r]
        wk = [self._key(k) for k in w]
        for k in rk:
            if k.startswith("bank") and k not in wk:
                wk.append(k)
        rk = [k for k in rk if not k.startswith("bank")]
        self.ops.append((eng, fn, rk, wk, False))

    @staticmethod
    def _is_dram(ap):
        return "DRam" in type(ap.tensor).__name__

    def dma(self, q, out, in_, r=None, w=None, **kw):
        if r is None:
            r = [] if self._is_dram(in_) else [in_]
        if w is None:
            w = [] if self._is_dram(out) else [out]
        self.ops.append((q, lambda e: e.dma_start(out=out, in_=in_, **kw),
                         [self._key(k) for k in r], [self._key(k) for k in w], True))

    def mm(self, out, lhsT, rhs, start=True, stop=True, r=None, w=None):
        r = [lhsT, rhs] if r is None else r
        w = [out] if w is None else w
        self.op("pe", lambda e: e.matmul(out, lhsT, rhs, start=start, stop=stop), r, w)

    def tr(self, out, in_, ident, r=None, w=None):
        r = [in_, ident] if r is None else r
        w = [out] if w is None else w
        self.op("pe", lambda e: e.transpose(out, in_, ident), r, w)

    def act(self, out, in_, func, bias=None, scale=None, accum_out=None, r=None, w=None):
        kw = {}
        rr = [in_]
        if bias is not None:
            kw["bias"] = bias
            if not isinstance(bias, (int, float)):
                rr.append(bias)
        if scale is not None:
            kw["scale"] = scale
            if not isinstance(scale, (int, float)):
                rr.append(scale)
        ww = [out]
        if accum_out is not None:
            kw["accum_out"] = accum_out
            ww.append(accum_out)
        r = rr if r is None else r
        w = ww if w is None else w
        self.op("act", lambda e: e.activation(out, in_, func, **kw), r, w)

    def tt(self, out, in0, in1, op, eng="dve", r=None, w=None):
        r = [in0, in1] if r is None else r
        w = [out] if w is None else w
        self.op(eng, lambda e: e.tensor_tensor(out, in0, in1, op), r, w)

    def ts(self, out, in0, s1, s2, op0, op1=None, eng="dve", r=None, w=None):
        rr = [in0] + [s for s in (s1, s2) if s is not None and not isinstance(s, (int, float))]
        r = rr if r is None else r
        w = [out] if w is None else w
        if op1 is None:
            self.op(eng, lambda e: e.tensor_scalar(out, in0, s1, None, op0), r, w)
        else:
            self.op(eng, lambda e: e.tensor_scalar(out, in0, s1, s2, op0, op1), r, w)

    def stt(self, out, in0, scalar, in1, op0, op1, r=None, w=None):
        rr = [in0, in1] + ([scalar] if not isinstance(scalar, (int, float)) else [])
        r = rr if r is None else r
        w = [out] if w is None else w
        self.op("dve", lambda e: e.scalar_tensor_tensor(out, in0, scalar, in1, op0, op1), r, w)

    def copy(self, out, in_, eng="dve", r=None, w=None):
        r = [in_] if r is None else r
        w = [out] if w is None else w
        if eng == "act":
            self.op("act", lambda e: e.copy(out, in_), r, w)
        else:
            self.op(eng, lambda e: e.tensor_copy(out, in_), r, w)

    def memset(self, ap, val, eng="pool"):
        self.op(eng, lambda e: e.memset(ap, val), [], [ap])

    def recip(self, out, in_):
        self.op("dve", lambda e: e.reciprocal(out, in_), [in_], [out])

    def rsqrt(self, out, in_, bias=0.0):
        self.act(out, in_, AF.Sqrt, bias=float(bias))
        self.recip(out, out)

    def red(self, out, in_, op, axis=None, r=None, w=None):
        axis = AX.X if axis is None else axis
        r = [in_] if r is None else r
        w = [out] if w is None else w
        self.op("dve", lambda e: e.tensor_reduce(out, in_, axis, op), r, w)

    def emit(self):
        nc = self.nc
        cnt = {e: 0 for e in ENGS}
        dcnt = {q: 0 for q in DMAQ}
        last_w = {}
        readers = {}
        plan = {e: [] for e in ENGS}
        latest = {}
        for (eng, fn, rs, ws, is_dma) in self.ops:
            if eng == "*":
                deps = {k + (v,) for k, v in latest.items()}
                for e in ENGS:
                    plan[e].append((None, deps, None))
                last_w.clear()
                readers.clear()
                continue
            deps = set()
            for k in rs:
                if k in last_w:
                    deps.add(last_w[k])
            for k in ws:
                if k in last_w:
                    deps.add(last_w[k])
                for t in readers.get(k, ()):
                    deps.add(t)
            if is_dma:
                m = dcnt[eng]
                dcnt[eng] += 1
                tok = ("d", eng, m % NDMA_SLOTS, 16 * (m // NDMA_SLOTS + 1))
                if m >= NDMA_SLOTS:
                    deps.add(("d", eng, m % NDMA_SLOTS, 16 * (m // NDMA_SLOTS)))
            else:
                n = cnt[eng]
                cnt[eng] += 1
                tok = ("c", eng, n // EPOCH, n % EPOCH + 1)
            latest[tok[:3]] = tok[3]
            if not self.same_engine_sync:
                deps = {d for d in deps if not (d[0] == "c" and d[1] == eng)}
            if eng == "pe" and not is_dma:
                deps = {d for d in deps if not (d[0] == "c" and d[1] == "pe")}
            plan[eng].append((fn, deps, tok))
            for k in ws:
                last_w[k] = tok
                readers[k] = []
            for k in rs:
                if k not in ws:
                    readers.setdefault(k, []).append(tok)
        stack = contextlib.ExitStack()
        sems = {}
        for e in ENGS:
            for ep in range(cnt[e] // EPOCH + 1):
                sems[("c", e, ep)] = stack.enter_context(nc.semaphore(f"s_{e}_{ep}"))
        for q in DMAQ:
            for s in range(NDMA_SLOTS):
                sems[("d", q, s)] = stack.enter_context(nc.semaphore(f"d_{q}_{s}"))
        self.n_instr = sum(len(v) for v in plan.values())
        self.counts = dict(cnt)
        self.dcounts = dict(dcnt)
        block = stack.enter_context(nc.Block())

        def run(engname, e):
            seen = {}
            for (fn, deps, tok) in plan[engname]:
                need = {}
                for d in deps:
                    key = d[:3]
                    if fn is None and key[0] == "c" and key[1] == engname:
                        continue
                    need[key] = max(need.get(key, 0), d[3])
                for key, v in sorted(need.items()):
                    if seen.get(key, 0) >= v:
                        continue
                    e.wait_ge(sems[key], v)
                    seen[key] = v
                if fn is None:
                    continue
                ins = fn(e)
                ins.then_inc(sems[tok[:3]], 16 if tok[0] == "d" else 1)
            if engname in DMAQ:
                m = dcnt[engname]
                for s in range(min(m, NDMA_SLOTS)):
                    total = (m - s + NDMA_SLOTS - 1) // NDMA_SLOTS
                    e.wait_ge(sems[("d", engname, s)], 16 * total)

        @block.tensor
        def _(e):
            run("pe", e)

        @block.scalar
        def _(e):
            run("act", e)

        @block.vector
        def _(e):
            run("dve", e)

        @block.gpsimd
        def _(e):
            run("pool", e)

        @block.sync
        def _(e):
            run("sp", e)

        stack.close()
        return nc


D = 1024
KC = 8
D_IN = 4896
EPS = 1e-6
NEXP = 32
FF = 512

PP = {}
_o = 0
for _n, _w in [("g_mix", 8), ("g_ffn", 8), ("b_gate", 24), ("conv_a", 12), ("gdn_conv", 48),
               ("qk_gain", 4), ("cmp_pe", 32), ("b_rt", 36), ("a_log", 4), ("dt_bias", 4),
               ("out_gain", 128)]:
    PP[_n] = (_o, _w)
    _o += _w
NPP = _o

FM_IN_COLS = [c * 128 for c in range(22)] + [2840 + c * 128 for c in range(12)]
N_FM = len(FM_IN_COLS)
TM_COLS = [(2048 + 3 * 128, 128), (2048 + 5 * 128, 128), (2816, 24), (4376, 512), (4888, 8)]
TM_OFF = {"slc_v": 0, "win_v": 128, "ng": 256, "z": 280, "ba": 792}
N_TM = 800


def pp_ap(pp, name, lo=0, n=None):
    o, w = PP[name]
    n = w - lo if n is None else n
    return pp[:, o + lo:o + lo + n]


class Ctx:
    pass


ALL_STAGES = ("p1", "conv", "gdn", "nsa", "merge", "moe")


class V:
    def __init__(self, t, i):
        self.t, self.i = t, i

    def ap(self):
        return self.t.ap()[self.i]


def build(T, L=1, S=1, stages=ALL_STAGES):
    P = Prog()
    nc = P.nc
    NT = T // 512
    c = Ctx()
    c.P, c.T, c.NT = P, T, NT
    xT_d = P.dram("xT", [S, D, T], F32, kind="ExternalInput")
    pp_d = P.dram("pp", [L, 128, NPP], F32, kind="ExternalInput")
    cst_d = P.dram("cst", [128, 1024], F32, kind="ExternalInput")
    w_in_d = P.dram("w_in", [L, D, D_IN], F32, kind="ExternalInput")
    w_gate_d = P.dram("w_gate", [L, D, 3 * D], F32, kind="ExternalInput")
    w_branch_d = P.dram("w_branch", [L, 3, 512, D], F32, kind="ExternalInput")
    w_out_d = P.dram("w_out", [L, D, D], F32, kind="ExternalInput")
    w_rt_d = P.dram("w_rt", [L, D, 36], F32, kind="ExternalInput")
    w_eg_d = P.dram("w_eg", [L, NEXP, D, FF], F32, kind="ExternalInput")
    w_eu_d = P.dram("w_eu", [L, NEXP, D, FF], F32, kind="ExternalInput")
    w_ed_d = P.dram("w_ed", [L, NEXP, FF, D], F32, kind="ExternalInput")
    cw1_d = P.dram("cmp_w1", [L, 2, 2048, 256], F32, kind="ExternalInput")
    cw2_d = P.dram("cmp_w2", [L, 2, 256, 64], F32, kind="ExternalInput")
    yT_out_d = P.dram("yT_out", [S, D, T], F32, kind="ExternalOutput")
    c.nsa_dc = nsa_dram_consts(P, T)
    uT = P.dram("uT", [N_FM * 128, T], F32)
    gT = P.dram("gT", [3 * D, T], F32)
    uTM = P.dram("uTM", [T, N_TM], F32)
    yT = P.dram("yT", [1536, T], BF16)
    x1T = P.dram("x1T", [D, T], F32)
    qkvTM = P.dram("qkvTM", [T, 1536], F32)
    xbuf = P.dram("xbuf", [2 * S, D, T], F32) if L > 1 else None
    c.uT, c.gT, c.uTM, c.yT, c.x1T = uT, gT, uTM, yT, x1T
    c.banks = P.banks

    pp = P.sb("pp", [128, NPP], F32)
    cst = P.sb("cst", [128, 1024], F32)
    g32 = P.sb("g32", [128, 16], F32)
    ones_bf = P.sb("ones_bf", [128, 128], BF16)
    ident = cst[:, 0:128]
    P.dma("sp", cst[:], cst_d.ap())
    P.memset(ones_bf[:], 1.0)
    c.pp, c.cst, c.ident, c.ones_bf, c.g32 = pp, cst, ident, ones_bf, g32
    for l in range(L):
        P.dma("sp", pp[:], pp_d.ap()[l])
        P.ts(g32[:, 0:16], pp[:, 0:16], 32.0, None, ALU.mult)
        w_in, w_gate, w_branch, w_out, w_rt = V(w_in_d, l), V(w_gate_d, l), V(w_branch_d, l), V(w_out_d, l), V(w_rt_d, l)
        w_eg, w_eu, w_ed, cw1, cw2 = V(w_eg_d, l), V(w_eu_d, l), V(w_ed_d, l), V(cw1_d, l), V(cw2_d, l)
        for s in range(S):
            xT = V(xT_d, s) if l == 0 else V(xbuf, ((l - 1) % 2) * S + s)
            yT_out = V(yT_out_d, s) if l == L - 1 else V(xbuf, (l % 2) * S + s)
            banks = P.banks

            def norm_tile(x_t, h_bf, gcol0, sq, rstd, bank, h_f32=None):
                P.act(sq[:], x_t[:], AF.Square)
                for k in range(KC):
                    P.mm(bank[:], ones_bf[:], sq[:, k, :], start=(k == 0), stop=(k == KC - 1))
                P.rsqrt(rstd[:], bank[:], float(D * EPS))
                for k in range(KC):
                    P.stt(h_bf[:, k, :], x_t[:, k, :], g32[:, gcol0 + k:gcol0 + k + 1], rstd[:], ALU.mult, ALU.mult)
                    if h_f32 is not None:
                        P.stt(h_f32[:, k, :], x_t[:, k, :], g32[:, gcol0 + k:gcol0 + k + 1], rstd[:], ALU.mult, ALU.mult)

            xT_v = xT.ap().rearrange("(k p) t -> p k t", p=128)
            x1T_v = x1T.ap().rearrange("(k p) t -> p k t", p=128)
            yo_v = yT_out.ap().rearrange("(k p) t -> p k t", p=128)

            if "p1" in stages:
                m0 = P.mark()
                hT = P.sb("hT", [128, KC, T], BF16)
                ma = P.mark()
                xt = [P.sb(f"xt{i}", [128, KC, 512], F32) for i in range(2)]
                sq = P.sb("sq", [128, KC, 512], BF16)
                rstd = P.sb("rstd", [128, 512], F32)
                for tt in range(NT):
                    x_t = xt[tt % 2]
                    P.dma("sp", x_t[:], xT_v[:, :, tt * 512:(tt + 1) * 512])
                    norm_tile(x_t, hT[:, :, tt * 512:(tt + 1) * 512], 0, sq, rstd, banks[0])
                P.release(ma)
                wst = [P.sb(f"wst{i}", [128, KC, 512], F32) for i in range(2)]
                wbf = [P.sb(f"wbf{i}", [128, KC, 512], BF16) for i in range(2)]
                ot = [P.sb(f"ot{i}", [128, 512], F32) for i in range(4)]
                w_in_v = w_in.ap().rearrange("(k p) n -> p k n", p=128)
                w_gate_v = w_gate.ap().rearrange("(k p) n -> p k n", p=128)
                groups = []
                cur = []
                for i, c0 in enumerate(FM_IN_COLS):
                    cur.append((w_in_v, c0, uT, i * 128, None))
                    if len(cur) == 4:
                        groups.append(cur)
                        cur = []
                if cur:
                    groups.append(cur)
                    cur = []
                for gch in range(24):
                    cur.append((w_gate_v, gch * 128, gT, gch * 128, gch))
                    if len(cur) == 4:
                        groups.append(cur)
                        cur = []
                cnt = 0
                for gi, grp in enumerate(groups):
                    st, wb = wst[gi % 2], wbf[gi % 2]
                    for j, (src, c0, dst, r0, gch) in enumerate(grp):
                        P.dma("sp", st[:, :, j * 128:(j + 1) * 128], src[:, :, c0:c0 + 128])
                    n = len(grp) * 128
                    P.copy(wb[:, :, 0:n], st[:, :, 0:n], eng="pool")
                    for tt in range(NT):
                        for j, (src, c0, dst, r0, gch) in enumerate(grp):
                            bank = banks[cnt % 4]
                            o_t = ot[cnt % 4]
                            cnt += 1
                            for k in range(KC):
                                P.mm(bank[:], wb[:, k, j * 128:(j + 1) * 128], hT[:, k, tt * 512:(tt + 1) * 512],
                                     start=(k == 0), stop=(k == KC - 1))
                            if gch is None:
                                P.copy(o_t[:], bank[:], eng="act")
                            else:
                                P.act(o_t[:], bank[:], AF.Sigmoid, bias=pp_ap(pp, "b_gate", gch, 1))
                            P.dma("pool", dst.ap()[r0:r0 + 128, tt * 512:(tt + 1) * 512], o_t[:])
                P.release(ma)
                wtm_st = P.sb("wtm_st", [128, KC, N_TM], F32)
                wtm = P.sb("wtm", [128, KC, N_TM], BF16)
                o = 0
                for (c0, n) in TM_COLS:
                    P.dma("sp", wtm_st[:, :, o:o + n], w_in_v[:, :, c0:c0 + n])
                    o += n
                P.copy(wtm[:], wtm_st[:], eng="pool")
                otm = [P.sb(f"otm{i}", [128, N_TM], F32) for i in range(2)]
                for st_i in range(T // 128):
                    o_t = otm[st_i % 2]
                    for (lo, hi, bank) in [(0, 512, banks[4]), (512, N_TM, banks[5])]:
                        for k in range(KC):
                            P.mm(bank[:, 0:hi - lo], hT[:, k, st_i * 128:(st_i + 1) * 128], wtm[:, k, lo:hi],
                                 start=(k == 0), stop=(k == KC - 1))
                        P.copy(o_t[:, lo:hi], bank[:, 0:hi - lo], eng="act")
                    P.dma("pool", uTM.ap()[st_i * 128:(st_i + 1) * 128, :], o_t[:])
                P.release(m0)

            if "conv" in stages:
                m0 = P.mark()
                bt = [P.sb(f"cb{i}", [128, 512], F32) for i in range(2)]
                ct = [P.sb(f"cc{i}", [128, 512], F32) for i in range(2)]
                xi = [P.sb(f"cx{i}", [128, 512], F32) for i in range(2)]
                cx = P.sb("cxh", [128, 2 + 512], F32)
                tmp = P.sb("ctmp", [128, 512], F32)
                yb = [P.sb(f"cy{i}", [128, 512], BF16) for i in range(2)]
                it = 0
                for j in range(4):
                    P.memset(cx[:, 0:2], 0.0, eng="dve")
                    for tt in range(NT):
                        b_t, c_t, x_t, y_t = bt[it % 2], ct[it % 2], xi[it % 2], yb[it % 2]
                        it += 1
                        sl = slice(tt * 512, (tt + 1) * 512)
                        P.dma("sp", b_t[:], uT.ap()[j * 128:(j + 1) * 128, sl])
                        P.dma("sp", c_t[:], uT.ap()[512 + j * 128:512 + (j + 1) * 128, sl])
                        P.dma("sp", x_t[:], uT.ap()[1024 + j * 128:1024 + (j + 1) * 128, sl])
                        P.tt(cx[:, 2:514], c_t[:], x_t[:], ALU.mult)
                        wc = lambda i: pp_ap(pp, "conv_a", j * 3 + i, 1)
                        P.ts(tmp[:], cx[:, 0:512], wc(0), None, ALU.mult)
                        P.stt(tmp[:], cx[:, 1:513], wc(1), tmp[:], ALU.mult, ALU.add)
                        P.stt(tmp[:], cx[:, 2:514], wc(2), tmp[:], ALU.mult, ALU.add)
                        P.tt(y_t[:], tmp[:], b_t[:], ALU.mult)
                        P.copy(cx[:, 0:2], cx[:, 512:514])
                        P.dma("pool", yT.ap()[j * 128:(j + 1) * 128, sl], y_t[:])
                P.release(m0)

            if "gdn" in stages:
                gdn_phase(c, qkvTM)
            if "nsa" in stages:
                nsa_phase(c, cw1, cw2)
            if "merge" in stages:
                m0 = P.mark()
                wbr = P.sb("wbr", [128, 12, D], BF16)
                wo = P.sb("wo", [128, KC, D], BF16)
                stg = [P.sb(f"mst{i}", [128, 4, D], F32) for i in range(2)]
                wbr_v = w_branch.ap().rearrange("r (k p) n -> p r k n", p=128)
                w_out_v = w_out.ap().rearrange("(k p) n -> p k n", p=128)
                for r in range(3):
                    P.dma("sp", stg[r % 2][:], wbr_v[:, r, :, :])
                    P.copy(wbr[:, r * 4:(r + 1) * 4, :], stg[r % 2][:], eng="pool")
                for hh in range(2):
                    P.dma("sp", stg[(hh + 1) % 2][:], w_out_v[:, hh * 4:(hh + 1) * 4, :])
                    P.copy(wo[:, hh * 4:(hh + 1) * 4, :], stg[(hh + 1) % 2][:], eng="pool")
                yt = [P.sb(f"myt{i}", [128, 12, 512], BF16) for i in range(2)]
                sg = [P.sb(f"msg{i}", [128, 3, 512], F32) for i in range(2)]
                mg = P.sb("mmg", [128, KC, 512], BF16)
                acc = P.sb("macc", [128, 512], F32)
                xt2 = [P.sb(f"mxt{i}", [128, KC, 512], F32) for i in range(2)]
                yT_v = yT.ap().rearrange("(k p) t -> p k t", p=128)
                gT_v = gT.ap().rearrange("(r m p) t -> p r m t", p=128, r=3)
                it = 0
                for tt in range(NT):
                    sl = slice(tt * 512, (tt + 1) * 512)
                    y_t = yt[tt % 2]
                    x_t = xt2[tt % 2]
                    P.dma("sp", y_t[:], yT_v[:, :, sl])
                    P.dma("sp", x_t[:], xT_v[:, :, sl])
                    for m in range(KC):
                        s_t = sg[it % 2]
                        it += 1
                        P.dma("sp", s_t[:], gT_v[:, :, m, sl])
                        for r in range(3):
                            bank = banks[r]
                            for k in range(4):
                                P.mm(bank[:], wbr[:, r * 4 + k, m * 128:(m + 1) * 128], y_t[:, r * 4 + k, :],
                                     start=(k == 0), stop=(k == 3))
                        P.tt(acc[:], banks[0][:], s_t[:, 0, :], ALU.mult)
                        P.tt(s_t[:, 1, :], banks[1][:], s_t[:, 1, :], ALU.mult)
                        P.tt(s_t[:, 2, :], banks[2][:], s_t[:, 2, :], ALU.mult)
                        P.tt(acc[:], acc[:], s_t[:, 1, :], ALU.add)
                        P.tt(mg[:, m, :], acc[:], s_t[:, 2, :], ALU.add)
                    for m in range(KC):
                        bank = banks[4 + m % 2]
                        for k in range(KC):
                            P.mm(bank[:], wo[:, k, m * 128:(m + 1) * 128], mg[:, k, :], start=(k == 0), stop=(k == KC - 1))
                        P.tt(x_t[:, m, :], x_t[:, m, :], bank[:], ALU.add)
                    P.dma("pool", x1T_v[:, :, sl], x_t[:])
                P.release(m0)

            if "moe" in stages:
                m0 = P.mark()
                TG = min(T, 2048)
                NG = T // TG
                NTG = TG // 512
                NSUB = TG // 128
                h2 = P.sb("h2", [128, KC, TG], BF16)
                wr = P.sb("wr", [128, KC, 36], F32)
                gates = P.sb("egates", [128, NSUB, 32], F32)
                acc = P.sb("eacc", [128, NSUB, D], F32)
                rt = P.sb("ert", [128, 160], F32)
                P.dma("sp", wr[:], w_rt.ap().rearrange("(k p) n -> p k n", p=128))
                b_rt = pp_ap(pp, "b_rt")
                stg_i = 0
                for g in range(NG):
                    ms = P.mark()
                    hf = P.sb("h2f", [128, KC, 512], F32)
                    xt3 = [P.sb(f"ext{i}", [128, KC, 512], F32) for i in range(1)]
                    sq = P.sb("esq", [128, KC, 512], BF16)
                    rstd = P.sb("erstd", [128, 512], F32)
                    for tt in range(NTG):
                        t0 = g * TG + tt * 512
                        x_t = xt3[0]
                        P.dma("sp", x_t[:], x1T_v[:, :, t0:t0 + 512])
                        norm_tile(x_t, h2[:, :, tt * 512:(tt + 1) * 512], 8, sq, rstd, banks[0], h_f32=hf)
                        for s4 in range(4):
                            si = tt * 4 + s4
                            lgb = banks[1]
                            for k in range(KC):
                                P.mm(lgb[:, 0:36], hf[:, k, s4 * 128:(s4 + 1) * 128], wr[:, k, :],
                                     start=(k == 0), stop=(k == KC - 1))
                            lg = rt[:, 0:36]
                            P.tt(lg, lgb[:, 0:36], b_rt, ALU.add)
                            mx = rt[:, 40:41]
                            P.red(mx, rt[:, 0:4], ALU.max)
                            oh = rt[:, 44:48]
                            P.ts(oh, rt[:, 0:4], mx, None, ALU.is_ge)
                            nmx = rt[:, 41:42]
                            P.ts(nmx, mx, -1.0, None, ALU.mult)
                            ex4 = rt[:, 48:52]
                            P.act(ex4, rt[:, 0:4], AF.Exp, bias=nmx)
                            s4s = rt[:, 42:43]
                            P.red(s4s, ex4, ALU.add)
                            pg = rt[:, 43:44]
                            P.recip(pg, s4s)
                            les = rt[:, 56:64]
                            P.ts(les, rt[:, 4:12], oh[:, 0:1], None, ALU.mult)
                            for gg in range(1, 4):
                                P.stt(les, rt[:, 4 + gg * 8:12 + gg * 8], oh[:, gg:gg + 1], les, ALU.mult, ALU.add)
                            top8 = rt[:, 64:72]
                            P.op("dve", lambda e, a=top8, b=les: e.max(a, b), [les], [top8])
                            m2 = rt[:, 72:80]
                            P.ts(m2, les, top8[:, 1:2], None, ALU.is_ge)
                            nm1 = rt[:, 80:81]
                            P.ts(nm1, top8[:, 0:1], -1.0, None, ALU.mult)
                            ex8 = rt[:, 88:96]
                            P.act(ex8, les, AF.Exp, bias=nm1)
                            w8 = rt[:, 96:104]
                            P.tt(w8, ex8, m2, ALU.mult)
                            den = rt[:, 81:82]
                            P.red(den, w8, ALU.add)
                            rden = rt[:, 82:83]
                            P.recip(rden, den)
                            P.ts(w8, w8, rden, None, ALU.mult)
                            P.ts(w8, w8, pg, None, ALU.mult)
                            for gg in range(4):
                                P.ts(gates[:, si, gg * 8:(gg + 1) * 8], w8, oh[:, gg:gg + 1], None, ALU.mult)
                    P.release(ms)
                    P.memset(acc[:], 0.0, eng="pool")
                    wstg = [P.sb(f"ewst{i}", [128, 4096], F32) for i in range(2)]
                    weg = [P.sb(f"eweg{i}", [128, KC, FF], BF16) for i in range(2)]
                    weu = [P.sb(f"eweu{i}", [128, KC, FF], BF16) for i in range(2)]
                    wed = [P.sb(f"ewed{i}", [128, 4, D], BF16) for i in range(2)]
                    sil = [P.sb(f"esil{i}", [128, 512], F32) for i in range(2)]
                    aT = [P.sb(f"eaT{i}", [128, 4, 512], BF16) for i in range(2)]
                    for e_i in range(NEXP):
                        b = e_i % 2
                        for (wsrc, wdst, view) in [
                            (w_eg, weg[b], "(k p) n -> p k n"), (w_eu, weu[b], "(k p) n -> p k n"),
                                (w_ed, wed[b], "(k p) n -> p k n")]:
                            st = wstg[stg_i % 2]
                            stg_i += 1
                            kk = 8 if wsrc is not w_ed else 4
                            nn = 4096 // kk
                            stv = st[:].rearrange("p (k n) -> p k n", k=kk)
                            P.dma("sp", stv, wsrc.ap()[e_i].rearrange(view, p=128))
                            P.copy(wdst[:], stv, eng="pool")
                        for tt in range(NTG):
                            hs = h2[:, :, tt * 512:(tt + 1) * 512]
                            a_t = aT[tt % 2]
                            for fc in range(4):
                                bg, bu = banks[(2 * fc) % 4], banks[(2 * fc + 1) % 4]
                                for k in range(KC):
                                    P.mm(bg[:], weg[b][:, k, fc * 128:(fc + 1) * 128], hs[:, k, :], start=(k == 0), stop=(k == KC - 1))
                                for k in range(KC):
                                    P.mm(bu[:], weu[b][:, k, fc * 128:(fc + 1) * 128], hs[:, k, :], start=(k == 0), stop=(k == KC - 1))
                                s_t = sil[fc % 2]
                                P.act(s_t[:], bg[:], AF.Silu)
                                P.tt(a_t[:, fc, :], s_t[:], bu[:], ALU.mult)
                            for s4 in range(4):
                                si = tt * 4 + s4
                                for hh in range(2):
                                    bank = banks[4 + (s4 * 2 + hh) % 4]
                                    for fc in range(4):
                                        P.mm(bank[:], a_t[:, fc, s4 * 128:(s4 + 1) * 128], wed[b][:, fc, hh * 512:(hh + 1) * 512],
                                             start=(fc == 0), stop=(fc == 3))
                                    P.stt(acc[:, si, hh * 512:(hh + 1) * 512], bank[:], gates[:, si, e_i:e_i + 1],
                                          acc[:, si, hh * 512:(hh + 1) * 512], ALU.mult, ALU.add)
                    P.release(ms)
                    xt3 = [P.sb(f"eot{i}", [128, KC, 512], F32) for i in range(2)]
                    for tt in range(NTG):
                        t0 = g * TG + tt * 512
                        x_t = xt3[tt % 2]
                        P.dma("sp", x_t[:], x1T_v[:, :, t0:t0 + 512])
                        for m in range(KC):
                            bank = banks[m % 4]
                            for s4 in range(4):
                                si = tt * 4 + s4
                                P.tr(bank[:, s4 * 128:(s4 + 1) * 128], acc[:, si, m * 128:(m + 1) * 128], ident)
                            P.tt(x_t[:, m, :], x_t[:, m, :], bank[:], ALU.add)
                        P.dma("pool", yo_v[:, :, t0:t0 + 512], x_t[:])
                    P.release(ms)
                P.release(m0)
            P.fence()
    return P, c


GQ0 = 22 * 128
C_TRI, C_NEGL, C_NEGU, C_OFFD, C_ONES = 128, 192, 256, 320, 384


def gdn_phase(c, qkvTM, upto=99):
    P, T, NT = c.P, c.T, c.NT
    pp, cst, ident, banks = c.pp, c.cst, c.ident, P.banks
    uT, uTM, yT = c.uT, c.uTM, c.yT
    m0 = P.mark()
    xin = [P.sb(f"g1x{i}", [128, 3 + 512], F32) for i in range(2)]
    tmp = P.sb("g1t", [128, 512], F32)
    sl_ = [P.sb(f"g1s{i}", [128, 512], F32) for i in range(2)]
    otm = [P.sb(f"g1o{i}", [128, 4, 128], F32) for i in range(2)]
    it = 0
    for ci in range(12):
        for tt in range(NT):
            x_t = xin[it % 2]
            s_t = sl_[it % 2]
            o_t = otm[it % 2]
            it += 1
            r0 = GQ0 + ci * 128
            if tt == 0:
                P.memset(x_t[:, 0:3], 0.0, eng="dve")
                P.dma("sp", x_t[:, 3:515], uT.ap()[r0:r0 + 128, 0:512])
            else:
                P.dma("sp", x_t[:, 0:515], uT.ap()[r0:r0 + 128, tt * 512 - 3:(tt + 1) * 512])
            wc = lambda i: pp_ap(pp, "gdn_conv", ci * 4 + i, 1)
            P.ts(tmp[:], x_t[:, 0:512], wc(0), None, ALU.mult)
            for i in range(1, 4):
                P.stt(tmp[:], x_t[:, i:i + 512], wc(i), tmp[:], ALU.mult, ALU.add)
            P.act(s_t[:], tmp[:], AF.Silu)
            bank = banks[it % 4]
            for s4 in range(4):
                P.tr(bank[:, s4 * 128:(s4 + 1) * 128], s_t[:, s4 * 128:(s4 + 1) * 128], ident)
            P.copy(o_t[:].rearrange("p a b -> p (a b)"), bank[:], eng="act")
            P.dma("pool", qkvTM.ap()[tt * 512:(tt + 1) * 512, ci * 128:(ci + 1) * 128].rearrange("(a p) f -> p a f", p=128), o_t[:])
    P.release(m0)

    if upto < 2:
        return
    m0 = P.mark()
    H = 4
    tri = cst[0:64, C_TRI:C_TRI + 64]
    negl = cst[0:64, C_NEGL:C_NEGL + 64]
    negu = cst[0:64, C_NEGU:C_NEGU + 64]
    offd = cst[0:64, C_OFFD:C_OFFD + 64]
    ones = cst[:, C_ONES:C_ONES + 128]
    id64 = cst[0:64, 0:64]
    qkv = [P.sb(f"gqkv{i}", [64, 1536], F32) for i in range(2)]
    zba = [P.sb(f"gzba{i}", [64, 520], F32) for i in range(2)]
    scs = [P.sb(f"gsc{i}", [128, 64], F32) for i in range(2)]
    expA = P.sb("gexpA", [64, 4], F32)
    P.act(expA[:], pp_ap(pp, "a_log")[0:64, :], AF.Exp)
    sq = P.sb("gsq", [64, 1024], F32)
    gz = P.sb("ggz", [64, 512], F32)
    yg = P.sb("gyg", [64, 512], F32)
    ygT = [P.sb(f"gygT{i}", [128, 4, 512], BF16) for i in range(2)]
    S = [P.sb(f"gS{h}", [128, 128], F32) for h in range(H)]
    hb = {}
    for h in range(H):
        for nm, shp in [("kn", [64, 128]), ("kb", [64, 128]), ("kbg", [64, 128]), ("kdec", [64, 128]),
                        ("qn", [64, 128]), ("qdec", [64, 128]), ("vb", [64, 128]),
                        ("knT", [128, 64]), ("kbT", [128, 64]), ("qnT", [128, 64]), ("qdecT", [128, 64]),
                        ("Tg", [64, 64]), ("nTg", [64, 64]), ("dec", [64, 64]), ("decT", [64, 64]),
                        ("N", [64, 64]), ("L", [64, 64]), ("N2", [64, 64]), ("L2", [64, 64]), ("IL", [64, 64]),
                        ("X", [64, 64]), ("X2", [64, 64]), ("attT", [64, 64]), ("u", [64, 128]), ("wT", [128, 64]),
                        ("vn", [64, 128]), ("ss", [64, 2]), ("osq", [64, 128])]:
            hb[(h, nm)] = P.sb(f"g{nm}{h}", shp, F32)
        P.memset(S[h][:], 0.0, eng="dve")

    def ps(h, b, c0, n, parts=64):
        return banks[2 * h + b][0:parts, c0:c0 + n], banks[2 * h + b]

    NCH = T // 64
    for ch in range(NCH):
        t0 = ch * 64
        q_t, z_t = qkv[ch % 2], zba[ch % 2]
        P.dma("sp", q_t[:], qkvTM.ap()[t0:t0 + 64, :])
        P.dma("sp", z_t[:], uTM.ap()[t0:t0 + 64, 280:800])
        sc = scs[ch % 2]
        beta, g, gc, eg, edec, dcol = sc[0:64, 0:4], sc[0:64, 4:8], sc[0:64, 8:12], sc[0:64, 12:16], sc[0:64, 16:20], sc[:, 20:24]
        rs, tmp4 = sc[0:64, 24:32], sc[0:64, 32:36]
        P.act(beta, z_t[:, 512:516], AF.Sigmoid)
        P.tt(tmp4, z_t[:, 516:520], pp_ap(pp, "dt_bias")[0:64, :], ALU.add)
        P.act(tmp4, tmp4, AF.Exp)
        P.act(tmp4, tmp4, AF.Ln, bias=1.0)
        P.tt(g, tmp4, expA[:], ALU.mult)
        P.ts(g, g, -1.0, None, ALU.mult)
        pgc, kgc = ps(0, 0, 0, 4)
        P.mm(pgc, tri, g, w=[kgc])
        pgl, kgl = ps(0, 0, 8, 4, parts=128)
        P.mm(pgl, ones[0:64, :], g, w=[kgl])
        P.copy(gc, pgc, r=[kgc])
        P.act(eg, pgc, AF.Exp, r=[kgc])
        P.act(dcol, pgl, AF.Exp, r=[kgl])
        P.tt(edec, pgl[0:64, :], gc, ALU.subtract, r=[kgl, gc])
        P.act(edec, edec, AF.Exp)
        if upto < 3:
            continue
        P.act(sq[:], q_t[:, 0:1024], AF.Square)
        P.red(rs, sq[:].rearrange("p (a b) -> p a b", b=128), ALU.add)
        P.rsqrt(rs, rs, EPS)
        P.act(gz[:], z_t[:, 0:512], AF.Silu)
        for h in range(H):
            P.tt(gz[:, h * 128:(h + 1) * 128], gz[:, h * 128:(h + 1) * 128], pp_ap(pp, "out_gain")[0:64, :], ALU.mult)
        if upto < 4:
            continue
        B = lambda h, nm: hb[(h, nm)]
        for h in range(H):
            qh, kh, vh = q_t[:, h * 128:(h + 1) * 128], q_t[:, 512 + h * 128:512 + (h + 1) * 128], q_t[:, 1024 + h * 128:1024 + (h + 1) * 128]
            P.ts(B(h, "kn")[:], kh, rs[:, 4 + h:5 + h], None, ALU.mult)
            P.ts(B(h, "kb")[:], B(h, "kn")[:], beta[:, h:h + 1], None, ALU.mult)
            P.ts(B(h, "kbg")[:], B(h, "kb")[:], eg[:, h:h + 1], None, ALU.mult)
            P.ts(B(h, "kdec")[:], B(h, "kn")[:], edec[:, h:h + 1], None, ALU.mult)
            P.ts(B(h, "qn")[:], qh, rs[:, h:h + 1], 128.0 ** -0.5, ALU.mult, ALU.mult)
            P.ts(B(h, "qdec")[:], B(h, "qn")[:], eg[:, h:h + 1], None, ALU.mult)
            P.ts(B(h, "vb")[:], vh, beta[:, h:h + 1], None, ALU.mult)
            P.ts(B(h, "Tg")[:], tri, g[:, h:h + 1], None, ALU.mult)
            P.ts(B(h, "nTg")[:], B(h, "Tg")[:], -1.0, None, ALU.mult)
        if upto < 5:
            continue
        for h in range(H):
            for i, (src, dst) in enumerate([("kn", "knT"), ("kb", "kbT"), ("qn", "qnT"), ("qdec", "qdecT")]):
                pt, kt = ps(h, 0, 64 + i * 64, 64, parts=128)
                P.tr(pt, B(h, src)[:], id64, w=[kt])
                P.copy(B(h, dst)[:], pt, eng="act", r=[kt])
        if upto < 6:
            continue
        for h in range(H):
            pD, kD = ps(h, 0, 320, 64)
            pDT, kDT = ps(h, 0, 384, 64)
            P.mm(pD, B(h, "Tg")[:], ones[0:64, 0:64], start=True, stop=False, w=[kD])
            P.mm(pD, ones[0:64, 0:64], B(h, "nTg")[:], start=False, stop=True, w=[kD])
            P.mm(pDT, ones[0:64, 0:64], B(h, "Tg")[:], start=True, stop=False, w=[kDT])
            P.mm(pDT, B(h, "nTg")[:], ones[0:64, 0:64], start=False, stop=True, w=[kDT])
            P.tt(B(h, "dec")[:], pD, negl, ALU.add, r=[kD, cst])
            P.tt(B(h, "decT")[:], pDT, negu, ALU.add, r=[kDT, cst])
            P.act(B(h, "dec")[:], B(h, "dec")[:], AF.Exp)
            P.act(B(h, "decT")[:], B(h, "decT")[:], AF.Exp)
        if upto < 7:
            continue
        for h in range(H):
            pKK, kKK = ps(h, 1, 0, 64)
            pKKT, kKKT = ps(h, 1, 64, 64)
            pQK, kQK = ps(h, 1, 128, 64)
            P.mm(pKK, B(h, "kbT")[:], B(h, "knT")[:], w=[kKK])
            P.mm(pKKT, B(h, "knT")[:], B(h, "kbT")[:], w=[kKKT])
            P.mm(pQK, B(h, "knT")[:], B(h, "qnT")[:], w=[kQK])
            P.tt(B(h, "L")[:], pKK, B(h, "dec")[:], ALU.mult, r=[kKK, B(h, "dec")])
            P.tt(B(h, "L")[:], B(h, "L")[:], offd, ALU.mult)
            P.tt(B(h, "N")[:], pKKT, B(h, "decT")[:], ALU.mult, r=[kKKT, B(h, "decT")])
            P.tt(B(h, "N")[:], B(h, "N")[:], offd, ALU.mult)
            P.tt(B(h, "attT")[:], pQK, B(h, "decT")[:], ALU.mult, r=[kQK, B(h, "decT")])
            P.tt(B(h, "X")[:], id64, B(h, "N")[:], ALU.subtract)
        if upto < 8:
            continue
        cur = {h: ("N", "L", "X") for h in range(H)}
        for it_ in range(5):
            for h in range(H):
                nN, nL, nX = cur[h]
                oN, oL, oX = ("N2", "L2", "X2") if nN == "N" else ("N", "L", "X")
                pN, kN = ps(h, 1, 192, 64)
                pL, kL = ps(h, 1, 256, 64)
                pX, kX = ps(h, 1, 320, 64)
                P.mm(pL, B(h, nN)[:], B(h, nL)[:], w=[kL])
                if it_ < 4:
                    P.mm(pN, B(h, nL)[:], B(h, nN)[:], w=[kN])
                    P.copy(B(h, oN)[:], pN, eng="act", r=[kN])
                    P.copy(B(h, oL)[:], pL, eng="act", r=[kL])
                P.tt(B(h, "IL")[:], pL, id64, ALU.add, r=[kL, cst])
                P.mm(pX, B(h, "IL")[:], B(h, nX)[:], w=[kX])
                P.copy(B(h, oX)[:], pX, r=[kX])
                cur[h] = (oN, oL, oX)
        if upto < 9:
            continue
        for h in range(H):
            X = B(h, cur[h][2])
            pu, ku = ps(h, 1, 384, 128)
            pw, kw = ps(h, 0, 448, 64, parts=128)
            P.mm(pu, X[:], B(h, "vb")[:], w=[ku])
            P.mm(pw, B(h, "kbg")[:], X[:], w=[kw])
            P.copy(B(h, "u")[:], pu, eng="act", r=[ku])
            P.copy(B(h, "wT")[:], pw, eng="act", r=[kw])
        if upto < 10:
            continue
        for h in range(H):
            pv, kv = ps(h, 1, 0, 128)
            po, ko = ps(h, 1, 128, 128)
            pS, kS = ps(h, 1, 384, 128, parts=128)
            P.mm(pv, B(h, "wT")[:], S[h][:], w=[kv])
            P.tt(B(h, "vn")[:], B(h, "u")[:], pv, ALU.subtract, r=[B(h, "u"), kv])
            P.mm(po, B(h, "qdecT")[:], S[h][:], start=True, stop=False, w=[ko])
            P.mm(po, B(h, "attT")[:], B(h, "vn")[:], start=False, stop=True, w=[ko])
            P.mm(pS, B(h, "kdec")[:], B(h, "vn")[:], w=[kS])
            P.stt(S[h][:], S[h][:], dcol[:, h:h + 1], pS, ALU.mult, ALU.add, r=[S[h], sc, kS])
            ssq = B(h, "ss")
            P.act(B(h, "osq")[:], po, AF.Square, r=[ko])
            P.red(ssq[:, 0:1], B(h, "osq")[:], ALU.add)
            P.act(ssq[:, 1:2], ssq[:, 0:1], AF.Sqrt, bias=EPS, scale=1.0 / 128)
            P.recip(ssq[:, 1:2], ssq[:, 1:2])
            P.stt(yg[:, h * 128:(h + 1) * 128], po, ssq[:, 1:2], gz[:, h * 128:(h + 1) * 128], ALU.mult, ALU.mult,
                  r=[ko, ssq, gz])
        if upto < 11:
            continue
        y_T = ygT[(ch // 8) % 2]
        for h in range(H):
            pt, kt = ps(h, 0, 448, 64, parts=128)
            P.tr(pt, yg[:, h * 128:(h + 1) * 128], id64, w=[kt])
            P.copy(y_T[:, h, (ch % 8) * 64:(ch % 8 + 1) * 64], pt, eng="act", r=[kt])
        if ch % 8 == 7:
            tt = ch // 8
            P.dma("pool", yT.ap()[1024:1536, tt * 512:(tt + 1) * 512].rearrange("(k p) t -> p k t", p=128), y_T[:])
    P.release(m0)


NQ0 = 1536
NKV0 = 2048
SLOPES = [2.0 ** (-(h + 1)) for h in range(8)]
NEGBIG = -30000.0


def nsa_consts(T):
    t = np.arange(T)
    qrows = np.zeros((8, 3, T), np.float32)
    for h in range(8):
        qrows[h, 0] = -SLOPES[h] * (t % 128)
        qrows[h, 1] = -SLOPES[h] * ((t % 512) // 128 * 128)
        qrows[h, 2] = SLOPES[h]
    krows = np.stack([np.ones(T), np.ones(T), (t % 128)]).astype(np.float32)
    kcrows = np.stack([np.ones(256), np.ones(256), 16.0 * (np.arange(256) % 128)]).astype(np.float32)
    bcol = np.zeros((128, 8 * 40), np.float32)
    for h in range(8):
        for m in range(-4, 36):
            bcol[:, h * 40 + m + 4] = -SLOPES[h] * 128.0 * m
    cbias = np.zeros((128, 128), np.float32)
    for h in range(8):
        for qi in range(8):
            for ci in range(2):
                cbias[:, h * 16 + qi * 2 + ci] = -SLOPES[h] * (512.0 * qi - 2048.0 * ci - 31.0)
    cmask = np.zeros((128, 9, 512), np.float32)
    cl = np.arange(128)[:, None]
    j = np.arange(512)[None, :]
    for i, d in enumerate([0, 512, 1024, 1536, 2048]):
        cmask[:, i] = np.where(d + j - 16 * cl - 31 >= 0, 0.0, NEGBIG)
    for i, d in enumerate([0, 512, 1024, 1536]):
        m = np.where(d + j - 16 * cl - 31 >= 0, 0.0, NEGBIG)
        m[127, :] = NEGBIG
        cmask[:, 5 + i] = m
    cc = np.arange(256)[:, None]
    jj = np.arange(64)[None, :]
    ov = ((16 * cc < 64 * jj + 64) & (16 * cc + 32 > 64 * jj)).astype(np.float32)
    ov[255] = 0
    ovl = ov.reshape(2, 128, 64).transpose(1, 0, 2)
    jt = (t // 64)[:, None]
    forced = (jj == 0) | (jj == jt) | (jj == jt - 1)
    selc = np.where(jj <= jt, np.where(forced, 1e6, 0.0), -1e30).astype(np.float32)
    selc = selc.reshape(T // 128, 128, 64).transpose(1, 0, 2)
    E = (t[None, :] // 64 == np.arange(64)[:, None]).astype(np.float32)
    kq = np.arange(128)
    tri_c = (kq[None, :] >= kq[:, None]).astype(np.float32)
    tri_w = (kq[None, :] < kq[:, None]).astype(np.float32)
    return dict(n_qrows=qrows.reshape(24, T), n_krows=krows, n_kcrows=kcrows, n_bcol=bcol, n_cbias=cbias,
                n_cmask=cmask.reshape(128, 9 * 512), n_ovl=np.ascontiguousarray(ovl).reshape(128, 128),
                n_selc=np.ascontiguousarray(selc).reshape(128, (T // 128) * 64), n_E=E,
                n_tri=np.concatenate([tri_c, tri_w], axis=1))


def nsa_dram_consts(P, T):
    dc = {}
    for nm, shp in [("n_qrows", [24, T]), ("n_krows", [3, T]), ("n_kcrows", [3, 256]), ("n_bcol", [128, 320]),
                    ("n_cbias", [128, 128]), ("n_cmask", [128, 9 * 512]), ("n_ovl", [128, 128]),
                    ("n_selc", [128, (T // 128) * 64]), ("n_E", [64, T]), ("n_tri", [128, 256])]:
        dc[nm] = P.dram(nm, shp, F32, kind="ExternalInput")
    return dc


def nsa_phase(c, cmp_w1, cmp_w2, upto=99):
    P, T, NT = c.P, c.T, c.NT
    pp, cst, ident, banks, ones_bf = c.pp, c.cst, c.ident, P.banks, c.ones_bf
    uT, uTM, yT = c.uT, c.uTM, c.yT
    NTT = T // 128
    NCT = 2 if T >= 4096 else 1
    NCMP = (T - 32) // 16 + 1
    assert NCMP <= 255
    dc = c.nsa_dc
    m0 = P.mark()
    Qaug = [P.sb(f"nQ{h}", [67, T], BF16) for h in range(8)]
    Kaug = {(br, g): P.sb(f"nK{br}{g}", [67, T], BF16) for br in range(2) for g in range(2)}
    Vaug = [P.sb(f"nV{br}", [128, NTT, 2, 66], BF16) for br in range(2)]
    Kc = [P.sb(f"nKc{g}", [67, 256], BF16) for g in range(2)]
    Vc = P.sb("nVc", [128, 2, 2, 66], BF16)
    gts = P.sb("ngts", [128, NTT, 24], F32)
    gq = P.sb("ngq", [64, 4], F32)
    P.copy(gq[:], pp_ap(pp, "qk_gain")[0:64, :])
    P.ts(gq[:, 0:1], gq[:, 0:1], 0.125, None, ALU.mult)
    m1 = P.mark()
    stg = P.sb("nstg", [128, T], F32)
    for h in range(8):
        P.dma("sp", stg[64:67, :], dc["n_qrows"].ap()[h * 3:(h + 1) * 3, :])
        P.copy(Qaug[h][64:67, :], stg[64:67, :])
    P.dma("sp", stg[64:67, :], dc["n_krows"].ap())
    for key in Kaug:
        P.copy(Kaug[key][64:67, :], stg[64:67, :])
    P.dma("sp", stg[64:67, 0:256], dc["n_kcrows"].ap())
    for g in range(2):
        P.copy(Kc[g][64:67, :], stg[64:67, 0:256])
        P.memset(Kc[g][0:64, :], 0.0, eng="dve")
    P.release(m1)
    for br in range(2):
        P.memset(Vaug[br][:], 1.0, eng="dve")
    P.memset(Vc[:], 1.0, eng="dve")

    m1 = P.mark()
    xin = [P.sb(f"nx{i}", [64, 512], F32) for i in range(3)]
    sq = [P.sb(f"nsq{i}", [64, 512], BF16) for i in range(2)]
    rstd = [P.sb(f"nrs{i}", [64, 512], F32) for i in range(2)]
    it = 0
    srcs = [(NQ0 + 64 * h, Qaug[h], 0) for h in range(8)]
    for br, s in ((0, 2), (1, 4)):
        for g in range(2):
            srcs.append((NKV0 + s * 128 + g * 64, Kaug[(br, g)], 2 + br))
    for tt in range(NT):
        for (r0, dst, gi) in srcs:
            x_t, s_t, r_t = xin[it % 3], sq[it % 2], rstd[it % 2]
            bank = banks[it % 2]
            it += 1
            P.dma("sp", x_t[:], uT.ap()[r0:r0 + 64, tt * 512:(tt + 1) * 512])
            P.act(s_t[:], x_t[:], AF.Square)
            P.mm(bank[0:64, :], ones_bf[0:64, 0:64], s_t[:])
            P.act(r_t[:], bank[0:64, :], AF.Sqrt, bias=EPS, scale=1.0 / 64)
            P.recip(r_t[:], r_t[:])
            P.stt(dst[0:64, tt * 512:(tt + 1) * 512], x_t[:], gq[:, gi:gi + 1], r_t[:], ALU.mult, ALU.mult)
    vt = [P.sb(f"nvt{i}", [128, 280], F32) for i in range(2)]
    for i in range(NTT):
        v_t = vt[i % 2]
        P.dma("sp", v_t[:], uTM.ap()[i * 128:(i + 1) * 128, 0:280])
        for br in range(2):
            P.copy(Vaug[br][:, i, :, 0:64], v_t[:, br * 128:(br + 1) * 128].rearrange("p (g d) -> p g d", g=2))
        P.act(gts[:, i, :], v_t[:, 256:280], AF.Sigmoid)
    P.release(m1)
    if upto < 2:
        P.release(m0)
        return

    m1 = P.mark()
    kv2 = P.sb("nkv2", [128, T + 16], F32)
    w1s = P.sb("nw1s", [128, 8, 256], F32)
    w1b = P.sb("nw1b", [128, 16, 256], BF16)
    w2s = P.sb("nw2s", [128, 2, 64], F32)
    w2b = P.sb("nw2b", [128, 2, 64], BF16)
    kvpe = P.sb("nkvpe", [128, 16, 256], BF16)
    hidT = P.sb("nhid", [128, 2, 256], BF16)
    g1 = P.sb("ng1", [128, 256], F32)
    g2 = P.sb("ng2", [128, 256], F32)
    ctm = P.sb("nctm", [128, 64], F32)
    csq = P.sb("ncsq", [128, 64], F32)
    cs = P.sb("ncs", [128, 4], F32)
    NC_ = NCMP
    for kv in range(2):
        for half in range(2):
            P.dma("sp", w1s[:], cmp_w1.ap()[kv].rearrange("(j p) n -> p j n", p=128)[:, half * 8:(half + 1) * 8, :])
            P.copy(w1b[:, half * 8:(half + 1) * 8, :], w1s[:], eng="pool")
        P.dma("sp", w2s[:], cmp_w2.ap()[kv].rearrange("(j p) n -> p j n", p=128))
        P.copy(w2b[:], w2s[:], eng="pool")
        for g in range(2):
            r0 = NKV0 + kv * 128 + g * 64
            P.memset(kv2[:, T - 1:T + 16], 0.0, eng="dve")
            P.dma("sp", kv2[0:64, 0:T], uT.ap()[r0:r0 + 64, 0:T])
            P.dma("sp", kv2[64:128, 0:T - 1], uT.ap()[r0:r0 + 64, 1:T])
            kvv = kv2[:, 0:T].rearrange("p (c s) -> p c s", s=16)
            P.memset(kvpe[:], 0.0, eng="pool")
            for j in range(16):
                if 2 * j < 16:
                    src = kvv[:, 0:NC_, 2 * j]
                else:
                    src = kvv[:, 1:NC_ + 1, 2 * j - 16]
                P.ts(kvpe[:, j, 0:NC_], src, pp_ap(pp, "cmp_pe", kv * 16 + j, 1), None, ALU.add)
            for hc in range(2):
                bank = banks[2 + hc]
                for j in range(16):
                    P.mm(bank[:, 0:256], w1b[:, j, hc * 128:(hc + 1) * 128], kvpe[:, j, :], start=(j == 0), stop=(j == 15))
                P.copy(g1[:], bank[:, 0:256])
                P.tt(g2[:], g1[:], g1[:], ALU.mult)
                P.ts(g2[:], g2[:], 0.044715, 1.0, ALU.mult, ALU.add)
                P.tt(g2[:], g2[:], g1[:], ALU.mult)
                P.act(g2[:], g2[:], AF.Sigmoid, scale=1.5957691216057308)
                P.tt(hidT[:, hc, :], g2[:], g1[:], ALU.mult)
            for ci in range(NCT):
                bank = banks[4 + ci]
                for hc in range(2):
                    P.mm(bank[:, 0:64], hidT[:, hc, ci * 128:(ci + 1) * 128], w2b[:, hc, :], start=(hc == 0), stop=(hc == 1))
                if kv == 0:
                    P.copy(ctm[:], bank[:, 0:64])
                    P.tt(csq[:], ctm[:], ctm[:], ALU.mult)
                    P.red(cs[:, 0:1], csq[:], ALU.add)
                    P.act(cs[:, 1:2], cs[:, 0:1], AF.Sqrt, bias=EPS, scale=1.0 / 64)
                    P.recip(cs[:, 1:2], cs[:, 1:2])
                    P.ts(ctm[:], ctm[:], cs[:, 1:2], None, ALU.mult)
                    P.tr(banks[6][0:64, 0:128], ctm[:], ident)
                    n = min(128, NC_ - ci * 128)
                    P.ts(Kc[g][0:64, ci * 128:ci * 128 + n], banks[6][0:64, 0:n], gq[:, 1:2], None, ALU.mult)
                else:
                    P.copy(Vc[:, ci, g, 0:64], bank[:, 0:64])
    P.release(m1)
    if upto < 3:
        P.release(m0)
        return

    m1 = P.mark()
    NEGMT = [P.sb(f"nNM{g}", [64, 512], BF16) for g in range(2)]
    bcol = P.sb("nbcol", [128, 320], F32)
    cbias = P.sb("ncbias", [128, 128], F32)
    cmask = P.sb("ncmask", [128, 9, 512], F32)
    ovl = P.sb("novl", [128, 2, 64], BF16)
    selc = P.sb("nselc", [128, NTT, 64], F32)
    Eb = P.sb("nE", [64, T], BF16)
    tri = P.sb("ntri", [128, 256], BF16)
    P.dma("sp", bcol[:], dc["n_bcol"].ap())
    P.dma("sp", cbias[:], dc["n_cbias"].ap())
    P.dma("sp", cmask[:].rearrange("p a b -> p (a b)"), dc["n_cmask"].ap())
    P.dma("sp", selc[:].rearrange("p a b -> p (a b)"), dc["n_selc"].ap())
    m2 = P.mark()
    stg = P.sb("nstg2", [128, T], F32)
    P.dma("sp", stg[0:64, :], dc["n_E"].ap())
    P.copy(Eb[:], stg[0:64, :])
    P.dma("sp", stg[:, 0:256], dc["n_tri"].ap())
    P.copy(tri[:], stg[:, 0:256])
    P.dma("sp", stg[:, 256:384], dc["n_ovl"].ap())
    P.copy(ovl[:].rearrange("p a b -> p (a b)"), stg[:, 256:384])
    P.release(m2)
    PT = [P.sb(f"nPT{i}", [128, 512], BF16) for i in range(3)]
    sm = [P.sb(f"nsm{i}", [128, 512], F32) for i in range(2)]
    Osb = [P.sb(f"nO{i}", [66, 512], F32) for i in range(2)]
    Isb = [P.sb(f"nI{i}", [64, 512], F32) for i in range(2)]
    acc = P.sb("nacc", [128, 4, 512], F32)
    imp = P.sb("nimp", [128, 4, 2, 64], F32)
    sc = P.sb("nsc", [128, 16], F32)
    sco = P.sb("nsco", [128, 64], F32)
    t8 = P.sb("nt8", [128, 8], F32)
    ngm = P.sb("nngm", [128, 64], F32)
    yTt = P.sb("nyT", [128, 4, 512], BF16)
    state = {"s": 0, "p": 0, "o": 0}
    SB = [banks[0], banks[1], banks[2]]
    OB = [banks[3], banks[4]]
    IB = banks[5]
    TB = banks[6]

    def finalize(o_sb, h, br, qi, first, i_sb=None):
        g = h // 4
        for s4 in range(4):
            P.tr(TB[:, 0:66], o_sb[:, s4 * 128:(s4 + 1) * 128], ident[0:66, 0:66])
            if i_sb is not None:
                P.tr(TB[:, 128:192], i_sb[:, s4 * 128:(s4 + 1) * 128], ident[0:64, 0:64])
            P.ts(sc[:, 0:1], TB[:, 64:65], 1e-30, None, ALU.add)
            P.recip(sc[:, 1:2], sc[:, 0:1])
            P.tt(sc[:, 2:3], sc[:, 1:2], gts[:, qi * 4 + s4, br * 8 + h:br * 8 + h + 1], ALU.mult)
            dst = acc[:, s4, h * 64:(h + 1) * 64]
            if first:
                P.ts(dst, TB[:, 0:64], sc[:, 2:3], None, ALU.mult)
            else:
                P.stt(dst, TB[:, 0:64], sc[:, 2:3], dst, ALU.mult, ALU.add)
            if i_sb is not None:
                idst = imp[:, s4, g, :]
                if h % 4 == 0:
                    P.ts(idst, TB[:, 128:192], sc[:, 1:2], None, ALU.mult)
                else:
                    P.stt(idst, TB[:, 128:192], sc[:, 1:2], idst, ALU.mult, ALU.add)

    for qi in range(NT):
        q0 = qi * 512
        qs = slice(q0, q0 + 512)
        for h in range(8):
            g = h // 4
            cts = [ci for ci in range(NCT) if 512 * qi - 2048 * ci + 511 >= 31]
            ob = OB[state["o"] % 2]
            o_sb, i_sb = Osb[state["o"] % 2], Isb[state["o"] % 2]
            state["o"] += 1
            pts = []
            for ci in cts:
                sb_ = SB[state["s"] % 3]
                state["s"] += 1
                p_t = PT[state["p"] % 3]
                state["p"] += 1
                s_m = sm[ci % 2]
                P.mm(sb_[:], Kc[g][0:67, ci * 128:(ci + 1) * 128], Qaug[h][0:67, qs])
                d = 512 * qi - 2048 * ci
                mi = None
                if ci == 0 and d <= 2048:
                    mi = d // 512
                elif ci == 1:
                    mi = 5 + d // 512
                if mi is not None:
                    P.tt(s_m[:], sb_[:], cmask[:, mi, :], ALU.add)
                    src = s_m
                else:
                    src = sb_
                P.act(p_t[:], src[:], AF.Exp, bias=cbias[:, h * 16 + qi * 2 + ci:h * 16 + qi * 2 + ci + 1])
                pts.append((ci, p_t))
            for n_, (ci, p_t) in enumerate(pts):
                P.mm(ob[0:66, :], Vc[:, ci, g, :], p_t[:], start=(n_ == 0), stop=(n_ == len(pts) - 1))
            for n_, (ci, p_t) in enumerate(pts):
                P.mm(IB[0:64, :], ovl[:, ci, :], p_t[:], start=(n_ == 0), stop=(n_ == len(pts) - 1))
            P.copy(o_sb[:], ob[0:66, :], eng="act")
            P.copy(i_sb[:], IB[0:64, :], eng="act")
            finalize(o_sb, h, 0, qi, True, i_sb)
        for g in range(2):
            for s4 in range(4):
                P.tt(sco[:], imp[:, s4, g, :], selc[:, qi * 4 + s4, :], ALU.add)
                P.op("dve", lambda e, a=t8, b=sco: e.max(a[:], b[:]), [sco], [t8])
                P.ts(ngm[:], sco[:], t8[:, 7:8], NEGBIG, ALU.is_lt, ALU.mult)
                P.tr(TB[0:64, 256:384], ngm[:], ident)
                P.copy(NEGMT[g][:, s4 * 128:(s4 + 1) * 128], TB[0:64, 256:384])
        for br in range(2):
            for h in range(8):
                g = h // 4
                ob = OB[state["o"] % 2]
                o_sb = Osb[state["o"] % 2]
                state["o"] += 1
                if br == 0:
                    kts = list(range(0, 4 * qi + 4))
                else:
                    kts = list(range(max(0, 4 * qi - 4), 4 * qi + 4))
                for n_, kt in enumerate(kts):
                    k0 = kt * 128
                    dlt = q0 - k0
                    lo = max(0, -dlt)
                    hi = 512 if br == 0 else min(512, 640 - dlt)
                    sb_ = SB[state["s"] % 3]
                    state["s"] += 1
                    p_t = PT[state["p"] % 3]
                    state["p"] += 1
                    P.mm(sb_[:, lo:hi], Kaug[(br, g)][0:67, k0:k0 + 128], Qaug[h][0:67, q0 + lo:q0 + hi],
                         start=True, stop=(br == 1))
                    if br == 0:
                        P.mm(sb_[:, lo:hi], Eb[:, k0:k0 + 128], NEGMT[g][:, lo:hi], start=False, stop=True)
                    bc = h * 40 + dlt // 128 + 4
                    P.act(p_t[:, lo:hi], sb_[:, lo:hi], AF.Exp, bias=bcol[:, bc:bc + 1])
                    if dlt <= 0:
                        P.tt(p_t[:, lo:lo + 128], p_t[:, lo:lo + 128], tri[:, 0:128], ALU.mult)
                    if br == 1 and dlt >= 128:
                        P.tt(p_t[:, hi - 128:hi], p_t[:, hi - 128:hi], tri[:, 128:256], ALU.mult)
                    P.mm(ob[0:66, lo:hi], Vaug[br][:, kt, g, :], p_t[:, lo:hi], start=(n_ == 0), stop=(n_ == len(kts) - 1))
                P.copy(o_sb[:], ob[0:66, :], eng="act")
                finalize(o_sb, h, 1 + br, qi, False)
        for m in range(4):
            for s4 in range(4):
                P.tr(TB[:, s4 * 128:(s4 + 1) * 128], acc[:, s4, m * 128:(m + 1) * 128], ident)
            P.copy(yTt[:, m, :], TB[:], eng="act")
        P.dma("pool", yT.ap()[512:1024, qs].rearrange("(k p) t -> p k t", p=128), yTt[:])
    P.release(m1)
    P.release(m0)


def make_pp(inp, l):
    pp = np.zeros((128, NPP), np.float32)

    def put(name, arr):
        o, w = PP[name]
        assert arr.shape == (128, w), (name, arr.shape)
        pp[:, o:o + w] = arr

    put("g_mix", inp["norm_mix"][l].reshape(8, 128).T)
    put("g_ffn", inp["norm_ffn"][l].reshape(8, 128).T)
    put("b_gate", inp["b_gate"][l].reshape(24, 128).T)
    put("conv_a", inp["conv_a_w"][l].reshape(3, 4, 128).transpose(2, 1, 0).reshape(128, 12))
    put("gdn_conv", inp["gdn_conv_w"][l].reshape(4, 12, 128).transpose(2, 1, 0).reshape(128, 48))
    put("qk_gain", np.tile(inp["nsa_qk_gain"][l].T, (2, 1)))
    put("cmp_pe", inp["cmp_pe"][l].reshape(2, 16, 2, 64).transpose(2, 3, 0, 1).reshape(128, 32))
    put("b_rt", np.tile(np.concatenate([inp["b_router_group"][l], inp["b_router_expert"][l]])[None, :], (128, 1)))
    put("a_log", np.tile(inp["gdn_a_log"][l][None, :], (128, 1)))
    put("dt_bias", np.tile(inp["gdn_dt_bias"][l][None, :], (128, 1)))
    put("out_gain", np.tile(inp["gdn_out_gain"][l][None, :], (128, 1)))
    return pp


def make_cst():
    c = np.zeros((128, 1024), np.float32)
    c[:, 0:128] = np.eye(128, dtype=np.float32)
    i = np.arange(64)
    c[0:64, 128:192] = (i[:, None] <= i[None, :])
    c[0:64, 192:256] = np.where(i[:, None] >= i[None, :], 0.0, -1e5)
    c[0:64, 256:320] = np.where(i[None, :] >= i[:, None], 0.0, -1e5)
    c[0:64, 320:384] = 1.0 - np.eye(64)
    c[:, 384:512] = 1.0
    return c


from concourse.bass_utils import run_bass_kernel_spmd

T_SEQ = 4096
N_CORES = 8
FUSED = False

_PROG_CACHE = {}


def _get_prog(L, S):
    key = (L, S)
    if key not in _PROG_CACHE:
        P, c = build(T_SEQ, L=L, S=S)
        _PROG_CACHE[key] = P.emit()
    return _PROG_CACHE[key]


def _weights(inp, layers):
    f = lambda a: np.ascontiguousarray(np.asarray(a, dtype=np.float32))
    sl = slice(layers[0], layers[-1] + 1)
    d = {
        "pp": f(np.stack([make_pp(inp, l) for l in layers])),
        "cst": make_cst(),
        "w_in": f(inp["w_in"][sl]),
        "w_gate": f(inp["w_gate"][sl]),
        "w_branch": f(inp["w_branch"][sl]),
        "w_out": f(inp["w_out"][sl]),
        "w_rt": f(np.concatenate([inp["w_router_group"][sl], inp["w_router_expert"][sl]], axis=2)),
        "w_eg": f(inp["w_expert_gate"][sl]),
        "w_eu": f(inp["w_expert_up"][sl]),
        "w_ed": f(inp["w_expert_down"][sl]),
        "cmp_w1": f(inp["cmp_w1"][sl]),
        "cmp_w2": f(inp["cmp_w2"][sl]),
    }
    d.update(nsa_consts(T_SEQ))
    return d


def kernel(**inputs):
    inp = {k: np.asarray(v) for k, v in inputs.items()}
    x = inp["x"].astype(np.float32, copy=False)
    B = x.shape[0]
    xT = np.ascontiguousarray(x.transpose(0, 2, 1))
    cores = list(range(N_CORES))
    if FUSED:
        S = B // N_CORES
        nc = _get_prog(4, S)
        w = _weights(inp, [0, 1, 2, 3])
        in_maps = [dict(w, xT=np.ascontiguousarray(xT[ci * S:(ci + 1) * S])) for ci in cores]
        res = run_bass_kernel_spmd(nc, in_maps, core_ids=cores)
        outT = np.concatenate([np.asarray(r["yT_out"]) for r in res.results], axis=0)
    else:
        nc = _get_prog(1, 1)
        cur = xT
        for l in range(4):
            w = _weights(inp, [l])
            nxt = np.empty_like(cur)
            for half in range(B // N_CORES):
                in_maps = [dict(w, xT=np.ascontiguousarray(cur[half * N_CORES + ci][None])) for ci in cores]
                res = run_bass_kernel_spmd(nc, in_maps, core_ids=cores)
                for ci in cores:
                    nxt[half * N_CORES + ci] = np.asarray(res.results[ci]["yT_out"])[0]
            cur = nxt
        outT = cur
    return np.ascontiguousarray(outT.transpose(0, 2, 1)).astype(np.float32, copy=False)
```

```python
import contextlib
import numpy as np
import concourse.bass as bass
import concourse.mybir as mybir

F32 = mybir.dt.float32
BF16 = mybir.dt.bfloat16
AF = mybir.ActivationFunctionType
ALU = mybir.AluOpType
AX = mybir.AxisListType

EPOCH = 30000
NDMA_SLOTS = 6
SB_BASE = 16512
SB_TOP = 229344
ENGS = ["pe", "act", "dve", "pool", "sp"]
DMAQ = ["sp", "pool"]
DTSIZE = {F32: 4, BF16: 2}


class Prog:
    def __init__(self, same_engine_sync=True):
        self.nc = bass.Bass("TRN2", target_bir_lowering=False)
        self.ops = []
        self.same_engine_sync = same_engine_sync
        self.sb_off = SB_BASE
        self.sb_peak = SB_BASE
        self.uid = 0
        self.banks = [self.nc.alloc_psum_tensor(f"bank{i}", [128, 512], F32) for i in range(8)]

    def dram(self, name, shape, dt, kind="Internal"):
        return self.nc.dram_tensor(name, list(shape), dt, kind=kind)

    def sb(self, name, shape, dt):
        n = 1
        for s in shape[1:]:
            n *= s
        nbytes = (n * DTSIZE[dt] + 31) // 32 * 32
        off = self.sb_off
        assert off + nbytes <= SB_TOP, f"SBUF overflow allocating {name}: {off}+{nbytes}"
        self.uid += 1
        t = self.nc.alloc_sbuf_tensor_at(f"{name}_{self.uid}", list(shape), dt, offset=off)
        self.sb_off = off + nbytes
        self.sb_peak = max(self.sb_peak, self.sb_off)
        return t

    def mark(self):
        return self.sb_off

    def release(self, mark):
        self.fence()
        self.sb_off = mark

    def fence(self):
        self.ops.append(("*", None, [], [], False))

    @staticmethod
    def _key(x):
        if isinstance(x, str):
            return x
        if isinstance(x, tuple):
            return Prog._key(x[0]) + "#" + str(x[1])
        t = getattr(x, "tensor", None)
        if t is not None:
            return t.name
        return x.name

    def op(self, eng, fn, r=(), w=()):
        rk = [self._key(k) for k in r]
        wk = [self._key(k) for k in w]
        for k in rk:
            if k.startswith("bank") and k not in wk:
                wk.append(k)
        rk = [k for k in rk if not k.startswith("bank")]
        self.ops.append((eng, fn, rk, wk, False))

    @staticmethod
    def _is_dram(ap):
        return "DRam" in type(ap.tensor).__name__

    def dma(self, q, out, in_, r=None, w=None, **kw):
        if r is None:
            r = [] if self._is_dram(in_) else [in_]
        if w is None:
            w = [] if self._is_dram(out) else [out]
        self.ops.append((q, lambda e: e.dma_start(out=out, in_=in_, **kw),
                         [self._key(k) for k in r], [self._key(k) for k in w], True))

    def mm(self, out, lhsT, rhs, start=True, stop=True, r=None, w=None):
        r = [lhsT, rhs] if r is None else r
        w = [out] if w is None else w
        self.op("pe", lambda e: e.matmul(out, lhsT, rhs, start=start, stop=stop), r, w)

    def tr(self, out, in_, ident, r=None, w=None):
        r = [in_, ident] if r is None else r
        w = [out] if w is None else w
        self.op("pe", lambda e: e.transpose(out, in_, ident), r, w)

    def act(self, out, in_, func, bias=None, scale=None, accum_out=None, r=None, w=None):
        kw = {}
        rr = [in_]
        if bias is not None:
            kw["bias"] = bias
            if not isinstance(bias, (int, float)):
                rr.append(bias)
        if scale is not None:
            kw["scale"] = scale
            if not isinstance(scale, (int, float)):
                rr.append(scale)
        ww = [out]
        if accum_out is not None:
            kw["accum_out"] = accum_out
            ww.append(accum_out)
        r = rr if r is None else r
        w = ww if w is None else w
        self.op("act", lambda e: e.activation(out, in_, func, **kw), r, w)

    def tt(self, out, in0, in1, op, eng="dve", r=None, w=None):
        r = [in0, in1] if r is None else r
        w = [out] if w is None else w
        self.op(eng, lambda e: e.tensor_tensor(out, in0, in1, op), r, w)

    def ts(self, out, in0, s1, s2, op0, op1=None, eng="dve", r=None, w=None):
        rr = [in0] + [s for s in (s1, s2) if s is not None and not isinstance(s, (int, float))]
        r = rr if r is None else r
        w = [out] if w is None else w
        if op1 is None:
            self.op(eng, lambda e: e.tensor_scalar(out, in0, s1, None, op0), r, w)
        else:
            self.op(eng, lambda e: e.tensor_scalar(out, in0, s1, s2, op0, op1), r, w)

    def stt(self, out, in0, scalar, in1, op0, op1, r=None, w=None):
        rr = [in0, in1] + ([scalar] if not isinstance(scalar, (int, float)) else [])
        r = rr if r is None else r
        w = [out] if w is None else w
        self.op("dve", lambda e: e.scalar_tensor_tensor(out, in0, scalar, in1, op0, op1), r, w)

    def copy(self, out, in_, eng="dve", r=None, w=None):
        r = [in_] if r is None else r
        w = [out] if w is None else w
        if eng == "act":
            self.op("act", lambda e: e.copy(out, in_), r, w)
        else:
            self.op(eng, lambda e: e.tensor_copy(out, in_), r, w)

    def memset(self, ap, val, eng="pool"):
        self.op(eng, lambda e: e.memset(ap, val), [], [ap])

    def recip(self, out, in_):
        self.op("dve", lambda e: e.reciprocal(out, in_), [in_], [out])

    def rsqrt(self, out, in_, bias=0.0):
        self.act(out, in_, AF.Sqrt, bias=float(bias))
        self.recip(out, out)

    def red(self, out, in_, op, axis=None, r=None, w=None):
        axis = AX.X if axis is None else axis
        r = [in_] if r is None else r
        w = [out] if w is None else w
        self.op("dve", lambda e: e.tensor_reduce(out, in_, axis, op), r, w)

    def emit(self):
        nc = self.nc
        cnt = {e: 0 for e in ENGS}
        dcnt = {q: 0 for q in DMAQ}
        last_w = {}
        readers = {}
        plan = {e: [] for e in ENGS}
        latest = {}
        for (eng, fn, rs, ws, is_dma) in self.ops:
            if eng == "*":
                deps = {k + (v,) for k, v in latest.items()}
                for e in ENGS:
                    plan[e].append((None, deps, None))
                last_w.clear()
                readers.clear()
                continue
            deps = set()
            for k in rs:
                if k in last_w:
                    deps.add(last_w[k])
            for k in ws:
                if k in last_w:
                    deps.add(last_w[k])
                for t in readers.get(k, ()):
                    deps.add(t)
            if is_dma:
                m = dcnt[eng]
                dcnt[eng] += 1
                tok = ("d", eng, m % NDMA_SLOTS, 16 * (m // NDMA_SLOTS + 1))
                if m >= NDMA_SLOTS:
                    deps.add(("d", eng, m % NDMA_SLOTS, 16 * (m // NDMA_SLOTS)))
            else:
                n = cnt[eng]
                cnt[eng] += 1
                tok = ("c", eng, n // EPOCH, n % EPOCH + 1)
            latest[tok[:3]] = tok[3]
            if not self.same_engine_sync:
                deps = {d for d in deps if not (d[0] == "c" and d[1] == eng)}
            if eng == "pe" and not is_dma:
                deps = {d for d in deps if not (d[0] == "c" and d[1] == "pe")}
            plan[eng].append((fn, deps, tok))
            for k in ws:
                last_w[k] = tok
                readers[k] = []
            for k in rs:
                if k not in ws:
                    readers.setdefault(k, []).append(tok)
        stack = contextlib.ExitStack()
        sems = {}
        for e in ENGS:
            for ep in range(cnt[e] // EPOCH + 1):
                sems[("c", e, ep)] = stack.enter_context(nc.semaphore(f"s_{e}_{ep}"))
        for q in DMAQ:
            for s in range(NDMA_SLOTS):
                sems[("d", q, s)] = stack.enter_context(nc.semaphore(f"d_{q}_{s}"))
        self.n_instr = sum(len(v) for v in plan.values())
        self.counts = dict(cnt)
        self.dcounts = dict(dcnt)
        block = stack.enter_context(nc.Block())

        def run(engname, e):
            seen = {}
            for (fn, deps, tok) in plan[engname]:
                need = {}
                for d in deps:
                    key = d[:3]
                    if fn is None and key[0] == "c" and key[1] == engname:
                        continue
                    need[key] = max(need.get(key, 0), d[3])
                for key, v in sorted(need.items()):
                    if seen.get(key, 0) >= v:
                        continue
                    e.wait_ge(sems[key], v)
                    seen[key] = v
                if fn is None:
                    continue
                ins = fn(e)
                ins.then_inc(sems[tok[:3]], 16 if tok[0] == "d" else 1)
            if engname in DMAQ:
                m = dcnt[engname]
                for s in range(min(m, NDMA_SLOTS)):
                    total = (m - s + NDMA_SLOTS - 1) // NDMA_SLOTS
                    e.wait_ge(sems[("d", engname, s)], 16 * total)

        @block.tensor
        def _(e):
            run("pe", e)

        @block.scalar
        def _(e):
            run("act", e)

        @block.vector
        def _(e):
            run("dve", e)

        @block.gpsimd
        def _(e):
            run("pool", e)

        @block.sync
        def _(e):
            run("sp", e)

        stack.close()
        return nc


D = 1024
KC = 8
D_IN = 4896
EPS = 1e-6
NEXP = 32
FF = 512

PP = {}
_o = 0
for _n, _w in [("g_mix", 8), ("g_ffn", 8), ("b_gate", 24), ("conv_a", 12), ("gdn_conv", 48),
               ("qk_gain", 4), ("cmp_pe", 32), ("b_rt", 36), ("a_log", 4), ("dt_bias", 4),
               ("out_gain", 128)]:
    PP[_n] = (_o, _w)
    _o += _w
NPP = _o

FM_IN_COLS = [c * 128 for c in range(22)] + [2840 + c * 128 for c in range(12)]
N_FM = len(FM_IN_COLS)
TM_COLS = [(2048 + 3 * 128, 128), (2048 + 5 * 128, 128), (2816, 24), (4376, 512), (4888, 8)]
TM_OFF = {"slc_v": 0, "win_v": 128, "ng": 256, "z": 280, "ba": 792}
N_TM = 800


def pp_ap(pp, name, lo=0, n=None):
    o, w = PP[name]
    n = w - lo if n is None else n
    return pp[:, o + lo:o + lo + n]


class Ctx:
    pass


ALL_STAGES = ("p1", "conv", "gdn", "nsa", "merge", "moe")


class V:
    def __init__(self, t, i):
        self.t, self.i = t, i

    def ap(self):
        return self.t.ap()[self.i]


def build(T, L=1, S=1, stages=ALL_STAGES):
    P = Prog()
    nc = P.nc
    NT = T // 512
    c = Ctx()
    c.P, c.T, c.NT = P, T, NT
    xT_d = P.dram("xT", [S, D, T], F32, kind="ExternalInput")
    pp_d = P.dram("pp", [L, 128, NPP], F32, kind="ExternalInput")
    cst_d = P.dram("cst", [128, 1024], F32, kind="ExternalInput")
    w_in_d = P.dram("w_in", [L, D, D_IN], F32, kind="ExternalInput")
    w_gate_d = P.dram("w_gate", [L, D, 3 * D], F32, kind="ExternalInput")
    w_branch_d = P.dram("w_branch", [L, 3, 512, D], F32, kind="ExternalInput")
    w_out_d = P.dram("w_out", [L, D, D], F32, kind="ExternalInput")
    w_rt_d = P.dram("w_rt", [L, D, 36], F32, kind="ExternalInput")
    w_eg_d = P.dram("w_eg", [L, NEXP, D, FF], F32, kind="ExternalInput")
    w_eu_d = P.dram("w_eu", [L, NEXP, D, FF], F32, kind="ExternalInput")
    w_ed_d = P.dram("w_ed", [L, NEXP, FF, D], F32, kind="ExternalInput")
    cw1_d = P.dram("cmp_w1", [L, 2, 2048, 256], F32, kind="ExternalInput")
    cw2_d = P.dram("cmp_w2", [L, 2, 256, 64], F32, kind="ExternalInput")
    yT_out_d = P.dram("yT_out", [S, D, T], F32, kind="ExternalOutput")
    c.nsa_dc = nsa_dram_consts(P, T)
    uT = P.dram("uT", [N_FM * 128, T], F32)
    gT = P.dram("gT", [3 * D, T], F32)
    uTM = P.dram("uTM", [T, N_TM], F32)
    yT = P.dram("yT", [1536, T], BF16)
    x1T = P.dram("x1T", [D, T], F32)
    qkvTM = P.dram("qkvTM", [T, 1536], F32)
    xbuf = P.dram("xbuf", [2 * S, D, T], F32) if L > 1 else None
    c.uT, c.gT, c.uTM, c.yT, c.x1T = uT, gT, uTM, yT, x1T
    c.banks = P.banks

    pp = P.sb("pp", [128, NPP], F32)
    cst = P.sb("cst", [128, 1024], F32)
    g32 = P.sb("g32", [128, 16], F32)
    ones_bf = P.sb("ones_bf", [128, 128], BF16)
    ident = cst[:, 0:128]
    P.dma("sp", cst[:], cst_d.ap())
    P.memset(ones_bf[:], 1.0)
    c.pp, c.cst, c.ident, c.ones_bf, c.g32 = pp, cst, ident, ones_bf, g32
    for l in range(L):
        P.dma("sp", pp[:], pp_d.ap()[l])
        P.ts(g32[:, 0:16], pp[:, 0:16], 32.0, None, ALU.mult)
        w_in, w_gate, w_branch, w_out, w_rt = V(w_in_d, l), V(w_gate_d, l), V(w_branch_d, l), V(w_out_d, l), V(w_rt_d, l)
        w_eg, w_eu, w_ed, cw1, cw2 = V(w_eg_d, l), V(w_eu_d, l), V(w_ed_d, l), V(cw1_d, l), V(cw2_d, l)
        for s in range(S):
            xT = V(xT_d, s) if l == 0 else V(xbuf, ((l - 1) % 2) * S + s)
            yT_out = V(yT_out_d, s) if l == L - 1 else V(xbuf, (l % 2) * S + s)
            banks = P.banks

            def norm_tile(x_t, h_bf, gcol0, sq, rstd, bank, h_f32=None):
                P.act(sq[:], x_t[:], AF.Square)
                for k in range(KC):
                    P.mm(bank[:], ones_bf[:], sq[:, k, :], start=(k == 0), stop=(k == KC - 1))
                P.rsqrt(rstd[:], bank[:], float(D * EPS))
                for k in range(KC):
                    P.stt(h_bf[:, k, :], x_t[:, k, :], g32[:, gcol0 + k:gcol0 + k + 1], rstd[:], ALU.mult, ALU.mult)
                    if h_f32 is not None:
                        P.stt(h_f32[:, k, :], x_t[:, k, :], g32[:, gcol0 + k:gcol0 + k + 1], rstd[:], ALU.mult, ALU.mult)

            xT_v = xT.ap().rearrange("(k p) t -> p k t", p=128)
            x1T_v = x1T.ap().rearrange("(k p) t -> p k t", p=128)
            yo_v = yT_out.ap().rearrange("(k p) t -> p k t", p=128)

            if "p1" in stages:
                m0 = P.mark()
                hT = P.sb("hT", [128, KC, T], BF16)
                ma = P.mark()
                xt = [P.sb(f"xt{i}", [128, KC, 512], F32) for i in range(2)]
                sq = P.sb("sq", [128, KC, 512], BF16)
                rstd = P.sb("rstd", [128, 512], F32)
                for tt in range(NT):
                    x_t = xt[tt % 2]
                    P.dma("sp", x_t[:], xT_v[:, :, tt * 512:(tt + 1) * 512])
                    norm_tile(x_t, hT[:, :, tt * 512:(tt + 1) * 512], 0, sq, rstd, banks[0])
                P.release(ma)
                wst = [P.sb(f"wst{i}", [128, KC, 512], F32) for i in range(2)]
                wbf = [P.sb(f"wbf{i}", [128, KC, 512], BF16) for i in range(2)]
                ot = [P.sb(f"ot{i}", [128, 512], F32) for i in range(4)]
                w_in_v = w_in.ap().rearrange("(k p) n -> p k n", p=128)
                w_gate_v = w_gate.ap().rearrange("(k p) n -> p k n", p=128)
                groups = []
                cur = []
                for i, c0 in enumerate(FM_IN_COLS):
                    cur.append((w_in_v, c0, uT, i * 128, None))
                    if len(cur) == 4:
                        groups.append(cur)
                        cur = []
                if cur:
                    groups.append(cur)
                    cur = []
                for gch in range(24):
                    cur.append((w_gate_v, gch * 128, gT, gch * 128, gch))
                    if len(cur) == 4:
                        groups.append(cur)
                        cur = []
                cnt = 0
                for gi, grp in enumerate(groups):
                    st, wb = wst[gi % 2], wbf[gi % 2]
                    for j, (src, c0, dst, r0, gch) in enumerate(grp):
                        P.dma("sp", st[:, :, j * 128:(j + 1) * 128], src[:, :, c0:c0 + 128])
                    n = len(grp) * 128
                    P.copy(wb[:, :, 0:n], st[:, :, 0:n], eng="pool")
                    for tt in range(NT):
                        for j, (src, c0, dst, r0, gch) in enumerate(grp):
                            bank = banks[cnt % 4]
                            o_t = ot[cnt % 4]
                            cnt += 1
                            for k in range(KC):
                                P.mm(bank[:], wb[:, k, j * 128:(j + 1) * 128], hT[:, k, tt * 512:(tt + 1) * 512],
                                     start=(k == 0), stop=(k == KC - 1))
                            if gch is None:
                                P.copy(o_t[:], bank[:], eng="act")
                            else:
                                P.act(o_t[:], bank[:], AF.Sigmoid, bias=pp_ap(pp, "b_gate", gch, 1))
                            P.dma("pool", dst.ap()[r0:r0 + 128, tt * 512:(tt + 1) * 512], o_t[:])
                P.release(ma)
                wtm_st = P.sb("wtm_st", [128, KC, N_TM], F32)
                wtm = P.sb("wtm", [128, KC, N_TM], BF16)
                o = 0
                for (c0, n) in TM_COLS:
                    P.dma("sp", wtm_st[:, :, o:o + n], w_in_v[:, :, c0:c0 + n])
                    o += n
                P.copy(wtm[:], wtm_st[:], eng="pool")
                otm = [P.sb(f"otm{i}", [128, N_TM], F32) for i in range(2)]
                for st_i in range(T // 128):
                    o_t = otm[st_i % 2]
                    for (lo, hi, bank) in [(0, 512, banks[4]), (512, N_TM, banks[5])]:
                        for k in range(KC):
                            P.mm(bank[:, 0:hi - lo], hT[:, k, st_i * 128:(st_i + 1) * 128], wtm[:, k, lo:hi],
                                 start=(k == 0), stop=(k == KC - 1))
                        P.copy(o_t[:, lo:hi], bank[:, 0:hi - lo], eng="act")
                    P.dma("pool", uTM.ap()[st_i * 128:(st_i + 1) * 128, :], o_t[:])
                P.release(m0)

            if "conv" in stages:
                m0 = P.mark()
                bt = [P.sb(f"cb{i}", [128, 512], F32) for i in range(2)]
                ct = [P.sb(f"cc{i}", [128, 512], F32) for i in range(2)]
                xi = [P.sb(f"cx{i}", [128, 512], F32) for i in range(2)]
                cx = P.sb("cxh", [128, 2 + 512], F32)
                tmp = P.sb("ctmp", [128, 512], F32)
                yb = [P.sb(f"cy{i}", [128, 512], BF16) for i in range(2)]
                it = 0
                for j in range(4):
                    P.memset(cx[:, 0:2], 0.0, eng="dve")
                    for tt in range(NT):
                        b_t, c_t, x_t, y_t = bt[it % 2], ct[it % 2], xi[it % 2], yb[it % 2]
                        it += 1
                        sl = slice(tt * 512, (tt + 1) * 512)
                        P.dma("sp", b_t[:], uT.ap()[j * 128:(j + 1) * 128, sl])
                        P.dma("sp", c_t[:], uT.ap()[512 + j * 128:512 + (j + 1) * 128, sl])
                        P.dma("sp", x_t[:], uT.ap()[1024 + j * 128:1024 + (j + 1) * 128, sl])
                        P.tt(cx[:, 2:514], c_t[:], x_t[:], ALU.mult)
                        wc = lambda i: pp_ap(pp, "conv_a", j * 3 + i, 1)
                        P.ts(tmp[:], cx[:, 0:512], wc(0), None, ALU.mult)
                        P.stt(tmp[:], cx[:, 1:513], wc(1), tmp[:], ALU.mult, ALU.add)
                        P.stt(tmp[:], cx[:, 2:514], wc(2), tmp[:], ALU.mult, ALU.add)
                        P.tt(y_t[:], tmp[:], b_t[:], ALU.mult)
                        P.copy(cx[:, 0:2], cx[:, 512:514])
                        P.dma("pool", yT.ap()[j * 128:(j + 1) * 128, sl], y_t[:])
                P.release(m0)

            if "gdn" in stages:
                gdn_phase(c, qkvTM)
            if "nsa" in stages:
                nsa_phase(c, cw1, cw2)
            if "merge" in stages:
                m0 = P.mark()
                wbr = P.sb("wbr", [128, 12, D], BF16)
                wo = P.sb("wo", [128, KC, D], BF16)
                stg = [P.sb(f"mst{i}", [128, 4, D], F32) for i in range(2)]
                wbr_v = w_branch.ap().rearrange("r (k p) n -> p r k n", p=128)
                w_out_v = w_out.ap().rearrange("(k p) n -> p k n", p=128)
                for r in range(3):
                    P.dma("sp", stg[r % 2][:], wbr_v[:, r, :, :])
                    P.copy(wbr[:, r * 4:(r + 1) * 4, :], stg[r % 2][:], eng="pool")
                for hh in range(2):
                    P.dma("sp", stg[(hh + 1) % 2][:], w_out_v[:, hh * 4:(hh + 1) * 4, :])
                    P.copy(wo[:, hh * 4:(hh + 1) * 4, :], stg[(hh + 1) % 2][:], eng="pool")
                yt = [P.sb(f"myt{i}", [128, 12, 512], BF16) for i in range(2)]
                sg = [P.sb(f"msg{i}", [128, 3, 512], F32) for i in range(2)]
                mg = P.sb("mmg", [128, KC, 512], BF16)
                acc = P.sb("macc", [128, 512], F32)
                xt2 = [P.sb(f"mxt{i}", [128, KC, 512], F32) for i in range(2)]
                yT_v = yT.ap().rearrange("(k p) t -> p k t", p=128)
                gT_v = gT.ap().rearrange("(r m p) t -> p r m t", p=128, r=3)
                it = 0
                for tt in range(NT):
                    sl = slice(tt * 512, (tt + 1) * 512)
                    y_t = yt[tt % 2]
                    x_t = xt2[tt % 2]
                    P.dma("sp", y_t[:], yT_v[:, :, sl])
                    P.dma("sp", x_t[:], xT_v[:, :, sl])
                    for m in range(KC):
                        s_t = sg[it % 2]
                        it += 1
                        P.dma("sp", s_t[:], gT_v[:, :, m, sl])
                        for r in range(3):
                            bank = banks[r]
                            for k in range(4):
                                P.mm(bank[:], wbr[:, r * 4 + k, m * 128:(m + 1) * 128], y_t[:, r * 4 + k, :],
                                     start=(k == 0), stop=(k == 3))
                        P.tt(acc[:], banks[0][:], s_t[:, 0, :], ALU.mult)
                        P.tt(s_t[:, 1, :], banks[1][:], s_t[:, 1, :], ALU.mult)
                        P.tt(s_t[:, 2, :], banks[2][:], s_t[:, 2, :], ALU.mult)
                        P.tt(acc[:], acc[:], s_t[:, 1, :], ALU.add)
                        P.tt(mg[:, m, :], acc[:], s_t[:, 2, :], ALU.add)
                    for m in range(KC):
                        bank = banks[4 + m % 2]
                        for k in range(KC):
                            P.mm(bank[:], wo[:, k, m * 128:(m + 1) * 128], mg[:, k, :], start=(k == 0), stop=(k == KC - 1))
                        P.tt(x_t[:, m, :], x_t[:, m, :], bank[:], ALU.add)
                    P.dma("pool", x1T_v[:, :, sl], x_t[:])
                P.release(m0)

            if "moe" in stages:
                m0 = P.mark()
                TG = min(T, 2048)
                NG = T // TG
                NTG = TG // 512
                NSUB = TG // 128
                h2 = P.sb("h2", [128, KC, TG], BF16)
                wr = P.sb("wr", [128, KC, 36], F32)
                gates = P.sb("egates", [128, NSUB, 32], F32)
                acc = P.sb("eacc", [128, NSUB, D], F32)
                rt = P.sb("ert", [128, 160], F32)
                P.dma("sp", wr[:], w_rt.ap().rearrange("(k p) n -> p k n", p=128))
                b_rt = pp_ap(pp, "b_rt")
                stg_i = 0
                for g in range(NG):
                    ms = P.mark()
                    hf = P.sb("h2f", [128, KC, 512], F32)
                    xt3 = [P.sb(f"ext{i}", [128, KC, 512], F32) for i in range(1)]
                    sq = P.sb("esq", [128, KC, 512], BF16)
                    rstd = P.sb("erstd", [128, 512], F32)
                    for tt in range(NTG):
                        t0 = g * TG + tt * 512
                        x_t = xt3[0]
                        P.dma("sp", x_t[:], x1T_v[:, :, t0:t0 + 512])
                        norm_tile(x_t, h2[:, :, tt * 512:(tt + 1) * 512], 8, sq, rstd, banks[0], h_f32=hf)
                        for s4 in range(4):
                            si = tt * 4 + s4
                            lgb = banks[1]
                            for k in range(KC):
                                P.mm(lgb[:, 0:36], hf[:, k, s4 * 128:(s4 + 1) * 128], wr[:, k, :],
                                     start=(k == 0), stop=(k == KC - 1))
                            lg = rt[:, 0:36]
                            P.tt(lg, lgb[:, 0:36], b_rt, ALU.add)
                            mx = rt[:, 40:41]
                            P.red(mx, rt[:, 0:4], ALU.max)
                            oh = rt[:, 44:48]
                            P.ts(oh, rt[:, 0:4], mx, None, ALU.is_ge)
                            nmx = rt[:, 41:42]
                            P.ts(nmx, mx, -1.0, None, ALU.mult)
                            ex4 = rt[:, 48:52]
                            P.act(ex4, rt[:, 0:4], AF.Exp, bias=nmx)
                            s4s = rt[:, 42:43]
                            P.red(s4s, ex4, ALU.add)
                            pg = rt[:, 43:44]
                            P.recip(pg, s4s)
                            les = rt[:, 56:64]
                            P.ts(les, rt[:, 4:12], oh[:, 0:1], None, ALU.mult)
                            for gg in range(1, 4):
                                P.stt(les, rt[:, 4 + gg * 8:12 + gg * 8], oh[:, gg:gg + 1], les, ALU.mult, ALU.add)
                            top8 = rt[:, 64:72]
                            P.op("dve", lambda e, a=top8, b=les: e.max(a, b), [les], [top8])
                            m2 = rt[:, 72:80]
                            P.ts(m2, les, top8[:, 1:2], None, ALU.is_ge)
                            nm1 = rt[:, 80:81]
                            P.ts(nm1, top8[:, 0:1], -1.0, None, ALU.mult)
                            ex8 = rt[:, 88:96]
                            P.act(ex8, les, AF.Exp, bias=nm1)
                            w8 = rt[:, 96:104]
                            P.tt(w8, ex8, m2, ALU.mult)
                            den = rt[:, 81:82]
                            P.red(den, w8, ALU.add)
                            rden = rt[:, 82:83]
                            P.recip(rden, den)
                            P.ts(w8, w8, rden, None, ALU.mult)
                            P.ts(w8, w8, pg, None, ALU.mult)
                            for gg in range(4):
                                P.ts(gates[:, si, gg * 8:(gg + 1) * 8], w8, oh[:, gg:gg + 1], None, ALU.mult)
                    P.release(ms)
                    P.memset(acc[:], 0.0, eng="pool")
                    wstg = [P.sb(f"ewst{i}", [128, 4096], F32) for i in range(2)]
                    weg = [P.sb(f"eweg{i}", [128, KC, FF], BF16) for i in range(2)]
                    weu = [P.sb(f"eweu{i}", [128, KC, FF], BF16) for i in range(2)]
                    wed = [P.sb(f"ewed{i}", [128, 4, D], BF16) for i in range(2)]
                    sil = [P.sb(f"esil{i}", [128, 512], F32) for i in range(2)]
                    aT = [P.sb(f"eaT{i}", [128, 4, 512], BF16) for i in range(2)]
                    for e_i in range(NEXP):
                        b = e_i % 2
                        for (wsrc, wdst, view) in [
                            (w_eg, weg[b], "(k p) n -> p k n"), (w_eu, weu[b], "(k p) n -> p k n"),
                                (w_ed, wed[b], "(k p) n -> p k n")]:
                            st = wstg[stg_i % 2]
                            stg_i += 1
                            kk = 8 if wsrc is not w_ed else 4
                            nn = 4096 // kk
                            stv = st[:].rearrange("p (k n) -> p k n", k=kk)
                            P.dma("sp", stv, wsrc.ap()[e_i].rearrange(view, p=128))
                            P.copy(wdst[:], stv, eng="pool")
                        for tt in range(NTG):
                            hs = h2[:, :, tt * 512:(tt + 1) * 512]
                            a_t = aT[tt % 2]
                            for fc in range(4):
                                bg, bu = banks[(2 * fc) % 4], banks[(2 * fc + 1) % 4]
                                for k in range(KC):
                                    P.mm(bg[:], weg[b][:, k, fc * 128:(fc + 1) * 128], hs[:, k, :], start=(k == 0), stop=(k == KC - 1))
                                for k in range(KC):
                                    P.mm(bu[:], weu[b][:, k, fc * 128:(fc + 1) * 128], hs[:, k, :], start=(k == 0), stop=(k == KC - 1))
                                s_t = sil[fc % 2]
                                P.act(s_t[:], bg[:], AF.Silu)
                                P.tt(a_t[:, fc, :], s_t[:], bu[:], ALU.mult)
                            for s4 in range(4):
                                si = tt * 4 + s4
                                for hh in range(2):
                                    bank = banks[4 + (s4 * 2 + hh) % 4]
                                    for fc in range(4):
                                        P.mm(bank[:], a_t[:, fc, s4 * 128:(s4 + 1) * 128], wed[b][:, fc, hh * 512:(hh + 1) * 512],
                                             start=(fc == 0), stop=(fc == 3))
                                    P.stt(acc[:, si, hh * 512:(hh + 1) * 512], bank[:], gates[:, si, e_i:e_i + 1],
                                          acc[:, si, hh * 512:(hh + 1) * 512], ALU.mult, ALU.add)
                    P.release(ms)
                    xt3 = [P.sb(f"eot{i}", [128, KC, 512], F32) for i in range(2)]
                    for tt in range(NTG):
                        t0 = g * TG + tt * 512
                        x_t = xt3[tt % 2]
                        P.dma("sp", x_t[:], x1T_v[:, :, t0:t0 + 512])
                        for m in range(KC):
                            bank = banks[m % 4]
                            for s4 in range(4):
                                si = tt * 4 + s4
                                P.tr(bank[:, s4 * 128:(s4 + 1) * 128], acc[:, si, m * 128:(m + 1) * 128], ident)
                            P.tt(x_t[:, m, :], x_t[:, m, :], bank[:], ALU.add)
                        P.dma("pool", yo_v[:, :, t0:t0 + 512], x_t[:])
                    P.release(ms)
                P.release(m0)
            P.fence()
    return P, c


GQ0 = 22 * 128
C_TRI, C_NEGL, C_NEGU, C_OFFD, C_ONES = 128, 192, 256, 320, 384


def gdn_phase(c, qkvTM, upto=99):
    P, T, NT = c.P, c.T, c.NT
    pp, cst, ident, banks = c.pp, c.cst, c.ident, P.banks
    uT, uTM, yT = c.uT, c.uTM, c.yT
    m0 = P.mark()
    xin = [P.sb(f"g1x{i}", [128, 3 + 512], F32) for i in range(2)]
    tmp = P.sb("g1t", [128, 512], F32)
    sl_ = [P.sb(f"g1s{i}", [128, 512], F32) for i in range(2)]
    otm = [P.sb(f"g1o{i}", [128, 4, 128], F32) for i in range(2)]
    it = 0
    for ci in range(12):
        for tt in range(NT):
            x_t = xin[it % 2]
            s_t = sl_[it % 2]
            o_t = otm[it % 2]
            it += 1
            r0 = GQ0 + ci * 128
            if tt == 0:
                P.memset(x_t[:, 0:3], 0.0, eng="dve")
                P.dma("sp", x_t[:, 3:515], uT.ap()[r0:r0 + 128, 0:512])
            else:
                P.dma("sp", x_t[:, 0:515], uT.ap()[r0:r0 + 128, tt * 512 - 3:(tt + 1) * 512])
            wc = lambda i: pp_ap(pp, "gdn_conv", ci * 4 + i, 1)
            P.ts(tmp[:], x_t[:, 0:512], wc(0), None, ALU.mult)
            for i in range(1, 4):
                P.stt(tmp[:], x_t[:, i:i + 512], wc(i), tmp[:], ALU.mult, ALU.add)
            P.act(s_t[:], tmp[:], AF.Silu)
            bank = banks[it % 4]
            for s4 in range(4):
                P.tr(bank[:, s4 * 128:(s4 + 1) * 128], s_t[:, s4 * 128:(s4 + 1) * 128], ident)
            P.copy(o_t[:].rearrange("p a b -> p (a b)"), bank[:], eng="act")
            P.dma("pool", qkvTM.ap()[tt * 512:(tt + 1) * 512, ci * 128:(ci + 1) * 128].rearrange("(a p) f -> p a f", p=128), o_t[:])
    P.release(m0)

    if upto < 2:
        return
    m0 = P.mark()
    H = 4
    tri = cst[0:64, C_TRI:C_TRI + 64]
    negl = cst[0:64, C_NEGL:C_NEGL + 64]
    negu = cst[0:64, C_NEGU:C_NEGU + 64]
    offd = cst[0:64, C_OFFD:C_OFFD + 64]
    ones = cst[:, C_ONES:C_ONES + 128]
    id64 = cst[0:64, 0:64]
    qkv = [P.sb(f"gqkv{i}", [64, 1536], F32) for i in range(2)]
    zba = [P.sb(f"gzba{i}", [64, 520], F32) for i in range(2)]
    scs = [P.sb(f"gsc{i}", [128, 64], F32) for i in range(2)]
    expA = P.sb("gexpA", [64, 4], F32)
    P.act(expA[:], pp_ap(pp, "a_log")[0:64, :], AF.Exp)
    sq = P.sb("gsq", [64, 1024], F32)
    gz = P.sb("ggz", [64, 512], F32)
    yg = P.sb("gyg", [64, 512], F32)
    ygT = [P.sb(f"gygT{i}", [128, 4, 512], BF16) for i in range(2)]
    S = [P.sb(f"gS{h}", [128, 128], F32) for h in range(H)]
    hb = {}
    for h in range(H):
        for nm, shp in [("kn", [64, 128]), ("kb", [64, 128]), ("kbg", [64, 128]), ("kdec", [64, 128]),
                        ("qn", [64, 128]), ("qdec", [64, 128]), ("vb", [64, 128]),
                        ("knT", [128, 64]), ("kbT", [128, 64]), ("qnT", [128, 64]), ("qdecT", [128, 64]),
                        ("Tg", [64, 64]), ("nTg", [64, 64]), ("dec", [64, 64]), ("decT", [64, 64]),
                        ("N", [64, 64]), ("L", [64, 64]), ("N2", [64, 64]), ("L2", [64, 64]), ("IL", [64, 64]),
                        ("X", [64, 64]), ("X2", [64, 64]), ("attT", [64, 64]), ("u", [64, 128]), ("wT", [128, 64]),
                        ("vn", [64, 128]), ("ss", [64, 2]), ("osq", [64, 128])]:
            hb[(h, nm)] = P.sb(f"g{nm}{h}", shp, F32)
        P.memset(S[h][:], 0.0, eng="dve")

    def ps(h, b, c0, n, parts=64):
        return banks[2 * h + b][0:parts, c0:c0 + n], banks[2 * h + b]

    NCH = T // 64
    for ch in range(NCH):
        t0 = ch * 64
        q_t, z_t = qkv[ch % 2], zba[ch % 2]
        P.dma("sp", q_t[:], qkvTM.ap()[t0:t0 + 64, :])
        P.dma("sp", z_t[:], uTM.ap()[t0:t0 + 64, 280:800])
        sc = scs[ch % 2]
        beta, g, gc, eg, edec, dcol = sc[0:64, 0:4], sc[0:64, 4:8], sc[0:64, 8:12], sc[0:64, 12:16], sc[0:64, 16:20], sc[:, 20:24]
        rs, tmp4 = sc[0:64, 24:32], sc[0:64, 32:36]
        P.act(beta, z_t[:, 512:516], AF.Sigmoid)
        P.tt(tmp4, z_t[:, 516:520], pp_ap(pp, "dt_bias")[0:64, :], ALU.add)
        P.act(tmp4, tmp4, AF.Exp)
        P.act(tmp4, tmp4, AF.Ln, bias=1.0)
        P.tt(g, tmp4, expA[:], ALU.mult)
        P.ts(g, g, -1.0, None, ALU.mult)
        pgc, kgc = ps(0, 0, 0, 4)
        P.mm(pgc, tri, g, w=[kgc])
        pgl, kgl = ps(0, 0, 8, 4, parts=128)
        P.mm(pgl, ones[0:64, :], g, w=[kgl])
        P.copy(gc, pgc, r=[kgc])
        P.act(eg, pgc, AF.Exp, r=[kgc])
        P.act(dcol, pgl, AF.Exp, r=[kgl])
        P.tt(edec, pgl[0:64, :], gc, ALU.subtract, r=[kgl, gc])
        P.act(edec, edec, AF.Exp)
        if upto < 3:
            continue
        P.act(sq[:], q_t[:, 0:1024], AF.Square)
        P.red(rs, sq[:].rearrange("p (a b) -> p a b", b=128), ALU.add)
        P.rsqrt(rs, rs, EPS)
        P.act(gz[:], z_t[:, 0:512], AF.Silu)
        for h in range(H):
            P.tt(gz[:, h * 128:(h + 1) * 128], gz[:, h * 128:(h + 1) * 128], pp_ap(pp, "out_gain")[0:64, :], ALU.mult)
        if upto < 4:
            continue
        B = lambda h, nm: hb[(h, nm)]
        for h in range(H):
            qh, kh, vh = q_t[:, h * 128:(h + 1) * 128], q_t[:, 512 + h * 128:512 + (h + 1) * 128], q_t[:, 1024 + h * 128:1024 + (h + 1) * 128]
            P.ts(B(h, "kn")[:], kh, rs[:, 4 + h:5 + h], None, ALU.mult)
            P.ts(B(h, "kb")[:], B(h, "kn")[:], beta[:, h:h + 1], None, ALU.mult)
            P.ts(B(h, "kbg")[:], B(h, "kb")[:], eg[:, h:h + 1], None, ALU.mult)
            P.ts(B(h, "kdec")[:], B(h, "kn")[:], edec[:, h:h + 1], None, ALU.mult)
            P.ts(B(h, "qn")[:], qh, rs[:, h:h + 1], 128.0 ** -0.5, ALU.mult, ALU.mult)
            P.ts(B(h, "qdec")[:], B(h, "qn")[:], eg[:, h:h + 1], None, ALU.mult)
            P.ts(B(h, "vb")[:], vh, beta[:, h:h + 1], None, ALU.mult)
            P.ts(B(h, "Tg")[:], tri, g[:, h:h + 1], None, ALU.mult)
            P.ts(B(h, "nTg")[:], B(h, "Tg")[:], -1.0, None, ALU.mult)
        if upto < 5:
            continue
        for h in range(H):
            for i, (src, dst) in enumerate([("kn", "knT"), ("kb", "kbT"), ("qn", "qnT"), ("qdec", "qdecT")]):
                pt, kt = ps(h, 0, 64 + i * 64, 64, parts=128)
                P.tr(pt, B(h, src)[:], id64, w=[kt])
                P.copy(B(h, dst)[:], pt, eng="act", r=[kt])
        if upto < 6:
            continue
        for h in range(H):
            pD, kD = ps(h, 0, 320, 64)
            pDT, kDT = ps(h, 0, 384, 64)
            P.mm(pD, B(h, "Tg")[:], ones[0:64, 0:64], start=True, stop=False, w=[kD])
            P.mm(pD, ones[0:64, 0:64], B(h, "nTg")[:], start=False, stop=True, w=[kD])
            P.mm(pDT, ones[0:64, 0:64], B(h, "Tg")[:], start=True, stop=False, w=[kDT])
            P.mm(pDT, B(h, "nTg")[:], ones[0:64, 0:64], start=False, stop=True, w=[kDT])
            P.tt(B(h, "dec")[:], pD, negl, ALU.add, r=[kD, cst])
            P.tt(B(h, "decT")[:], pDT, negu, ALU.add, r=[kDT, cst])
            P.act(B(h, "dec")[:], B(h, "dec")[:], AF.Exp)
            P.act(B(h, "decT")[:], B(h, "decT")[:], AF.Exp)
        if upto < 7:
            continue
        for h in range(H):
            pKK, kKK = ps(h, 1, 0, 64)
            pKKT, kKKT = ps(h, 1, 64, 64)
            pQK, kQK = ps(h, 1, 128, 64)
            P.mm(pKK, B(h, "kbT")[:], B(h, "knT")[:], w=[kKK])
            P.mm(pKKT, B(h, "knT")[:], B(h, "kbT")[:], w=[kKKT])
            P.mm(pQK, B(h, "knT")[:], B(h, "qnT")[:], w=[kQK])
            P.tt(B(h, "L")[:], pKK, B(h, "dec")[:], ALU.mult, r=[kKK, B(h, "dec")])
            P.tt(B(h, "L")[:], B(h, "L")[:], offd, ALU.mult)
            P.tt(B(h, "N")[:], pKKT, B(h, "decT")[:], ALU.mult, r=[kKKT, B(h, "decT")])
            P.tt(B(h, "N")[:], B(h, "N")[:], offd, ALU.mult)
            P.tt(B(h, "attT")[:], pQK, B(h, "decT")[:], ALU.mult, r=[kQK, B(h, "decT")])
            P.tt(B(h, "X")[:], id64, B(h, "N")[:], ALU.subtract)
        if upto < 8:
            continue
        cur = {h: ("N", "L", "X") for h in range(H)}
        for it_ in range(5):
            for h in range(H):
                nN, nL, nX = cur[h]
                oN, oL, oX = ("N2", "L2", "X2") if nN == "N" else ("N", "L", "X")
                pN, kN = ps(h, 1, 192, 64)
                pL, kL = ps(h, 1, 256, 64)
                pX, kX = ps(h, 1, 320, 64)
                P.mm(pL, B(h, nN)[:], B(h, nL)[:], w=[kL])
                if it_ < 4:
                    P.mm(pN, B(h, nL)[:], B(h, nN)[:], w=[kN])
                    P.copy(B(h, oN)[:], pN, eng="act", r=[kN])
                    P.copy(B(h, oL)[:], pL, eng="act", r=[kL])
                P.tt(B(h, "IL")[:], pL, id64, ALU.add, r=[kL, cst])
                P.mm(pX, B(h, "IL")[:], B(h, nX)[:], w=[kX])
                P.copy(B(h, oX)[:], pX, r=[kX])
                cur[h] = (oN, oL, oX)
        if upto < 9:
            continue
        for h in range(H):
            X = B(h, cur[h][2])
            pu, ku = ps(h, 1, 384, 128)
            pw, kw = ps(h, 0, 448, 64, parts=128)
            P.mm(pu, X[:], B(h, "vb")[:], w=[ku])
            P.mm(pw, B(h, "kbg")[:], X[:], w=[kw])
            P.copy(B(h, "u")[:], pu, eng="act", r=[ku])
            P.copy(B(h, "wT")[:], pw, eng="act", r=[kw])
        if upto < 10:
            continue
        for h in range(H):
            pv, kv = ps(h, 1, 0, 128)
            po, ko = ps(h, 1, 128, 128)
            pS, kS = ps(h, 1, 384, 128, parts=128)
            P.mm(pv, B(h, "wT")[:], S[h][:], w=[kv])
            P.tt(B(h, "vn")[:], B(h, "u")[:], pv, ALU.subtract, r=[B(h, "u"), kv])
            P.mm(po, B(h, "qdecT")[:], S[h][:], start=True, stop=False, w=[ko])
            P.mm(po, B(h, "attT")[:], B(h, "vn")[:], start=False, stop=True, w=[ko])
            P.mm(pS, B(h, "kdec")[:], B(h, "vn")[:], w=[kS])
            P.stt(S[h][:], S[h][:], dcol[:, h:h + 1], pS, ALU.mult, ALU.add, r=[S[h], sc, kS])
            ssq = B(h, "ss")
            P.act(B(h, "osq")[:], po, AF.Square, r=[ko])
            P.red(ssq[:, 0:1], B(h, "osq")[:], ALU.add)
            P.act(ssq[:, 1:2], ssq[:, 0:1], AF.Sqrt, bias=EPS, scale=1.0 / 128)
            P.recip(ssq[:, 1:2], ssq[:, 1:2])
            P.stt(yg[:, h * 128:(h + 1) * 128], po, ssq[:, 1:2], gz[:, h * 128:(h + 1) * 128], ALU.mult, ALU.mult,
                  r=[ko, ssq, gz])
        if upto < 11:
            continue
        y_T = ygT[(ch // 8) % 2]
        for h in range(H):
            pt, kt = ps(h, 0, 448, 64, parts=128)
            P.tr(pt, yg[:, h * 128:(h + 1) * 128], id64, w=[kt])
            P.copy(y_T[:, h, (ch % 8) * 64:(ch % 8 + 1) * 64], pt, eng="act", r=[kt])
        if ch % 8 == 7:
            tt = ch // 8
            P.dma("pool", yT.ap()[1024:1536, tt * 512:(tt + 1) * 512].rearrange("(k p) t -> p k t", p=128), y_T[:])
    P.release(m0)


NQ0 = 1536
NKV0 = 2048
SLOPES = [2.0 ** (-(h + 1)) for h in range(8)]
NEGBIG = -30000.0


def nsa_consts(T):
    t = np.arange(T)
    qrows = np.zeros((8, 3, T), np.float32)
    for h in range(8):
        qrows[h, 0] = -SLOPES[h] * (t % 128)
        qrows[h, 1] = -SLOPES[h] * ((t % 512) // 128 * 128)
        qrows[h, 2] = SLOPES[h]
    krows = np.stack([np.ones(T), np.ones(T), (t % 128)]).astype(np.float32)
    kcrows = np.stack([np.ones(256), np.ones(256), 16.0 * (np.arange(256) % 128)]).astype(np.float32)
    bcol = np.zeros((128, 8 * 40), np.float32)
    for h in range(8):
        for m in range(-4, 36):
            bcol[:, h * 40 + m + 4] = -SLOPES[h] * 128.0 * m
    cbias = np.zeros((128, 128), np.float32)
    for h in range(8):
        for qi in range(8):
            for ci in range(2):
                cbias[:, h * 16 + qi * 2 + ci] = -SLOPES[h] * (512.0 * qi - 2048.0 * ci - 31.0)
    cmask = np.zeros((128, 9, 512), np.float32)
    cl = np.arange(128)[:, None]
    j = np.arange(512)[None, :]
    for i, d in enumerate([0, 512, 1024, 1536, 2048]):
        cmask[:, i] = np.where(d + j - 16 * cl - 31 >= 0, 0.0, NEGBIG)
    for i, d in enumerate([0, 512, 1024, 1536]):
        m = np.where(d + j - 16 * cl - 31 >= 0, 0.0, NEGBIG)
        m[127, :] = NEGBIG
        cmask[:, 5 + i] = m
    cc = np.arange(256)[:, None]
    jj = np.arange(64)[None, :]
    ov = ((16 * cc < 64 * jj + 64) & (16 * cc + 32 > 64 * jj)).astype(np.float32)
    ov[255] = 0
    ovl = ov.reshape(2, 128, 64).transpose(1, 0, 2)
    jt = (t // 64)[:, None]
    forced = (jj == 0) | (jj == jt) | (jj == jt - 1)
    selc = np.where(jj <= jt, np.where(forced, 1e6, 0.0), -1e30).astype(np.float32)
    selc = selc.reshape(T // 128, 128, 64).transpose(1, 0, 2)
    E = (t[None, :] // 64 == np.arange(64)[:, None]).astype(np.float32)
    kq = np.arange(128)
    tri_c = (kq[None, :] >= kq[:, None]).astype(np.float32)
    tri_w = (kq[None, :] < kq[:, None]).astype(np.float32)
    return dict(n_qrows=qrows.reshape(24, T), n_krows=krows, n_kcrows=kcrows, n_bcol=bcol, n_cbias=cbias,
                n_cmask=cmask.reshape(128, 9 * 512), n_ovl=np.ascontiguousarray(ovl).reshape(128, 128),
                n_selc=np.ascontiguousarray(selc).reshape(128, (T // 128) * 64), n_E=E,
                n_tri=np.concatenate([tri_c, tri_w], axis=1))


def nsa_dram_consts(P, T):
    dc = {}
    for nm, shp in [("n_qrows", [24, T]), ("n_krows", [3, T]), ("n_kcrows", [3, 256]), ("n_bcol", [128, 320]),
                    ("n_cbias", [128, 128]), ("n_cmask", [128, 9 * 512]), ("n_ovl", [128, 128]),
                    ("n_selc", [128, (T // 128) * 64]), ("n_E", [64, T]), ("n_tri", [128, 256])]:
        dc[nm] = P.dram(nm, shp, F32, kind="ExternalInput")
    return dc


def nsa_phase(c, cmp_w1, cmp_w2, upto=99):
    P, T, NT = c.P, c.T, c.NT
    pp, cst, ident, banks, ones_bf = c.pp, c.cst, c.ident, P.banks, c.ones_bf
    uT, uTM, yT = c.uT, c.uTM, c.yT
    NTT = T // 128
    NCT = 2 if T >= 4096 else 1
    NCMP = (T - 32) // 16 + 1
    assert NCMP <= 255
    dc = c.nsa_dc
    m0 = P.mark()
    Qaug = [P.sb(f"nQ{h}", [67, T], BF16) for h in range(8)]
    Kaug = {(br, g): P.sb(f"nK{br}{g}", [67, T], BF16) for br in range(2) for g in range(2)}
    Vaug = [P.sb(f"nV{br}", [128, NTT, 2, 66], BF16) for br in range(2)]
    Kc = [P.sb(f"nKc{g}", [67, 256], BF16) for g in range(2)]
    Vc = P.sb("nVc", [128, 2, 2, 66], BF16)
    gts = P.sb("ngts", [128, NTT, 24], F32)
    gq = P.sb("ngq", [64, 4], F32)
    P.copy(gq[:], pp_ap(pp, "qk_gain")[0:64, :])
    P.ts(gq[:, 0:1], gq[:, 0:1], 0.125, None, ALU.mult)
    m1 = P.mark()
    stg = P.sb("nstg", [128, T], F32)
    for h in range(8):
        P.dma("sp", stg[64:67, :], dc["n_qrows"].ap()[h * 3:(h + 1) * 3, :])
        P.copy(Qaug[h][64:67, :], stg[64:67, :])
    P.dma("sp", stg[64:67, :], dc["n_krows"].ap())
    for key in Kaug:
        P.copy(Kaug[key][64:67, :], stg[64:67, :])
    P.dma("sp", stg[64:67, 0:256], dc["n_kcrows"].ap())
    for g in range(2):
        P.copy(Kc[g][64:67, :], stg[64:67, 0:256])
        P.memset(Kc[g][0:64, :], 0.0, eng="dve")
    P.release(m1)
    for br in range(2):
        P.memset(Vaug[br][:], 1.0, eng="dve")
    P.memset(Vc[:], 1.0, eng="dve")

    m1 = P.mark()
    xin = [P.sb(f"nx{i}", [64, 512], F32) for i in range(3)]
    sq = [P.sb(f"nsq{i}", [64, 512], BF16) for i in range(2)]
    rstd = [P.sb(f"nrs{i}", [64, 512], F32) for i in range(2)]
    it = 0
    srcs = [(NQ0 + 64 * h, Qaug[h], 0) for h in range(8)]
    for br, s in ((0, 2), (1, 4)):
        for g in range(2):
            srcs.append((NKV0 + s * 128 + g * 64, Kaug[(br, g)], 2 + br))
    for tt in range(NT):
        for (r0, dst, gi) in srcs:
            x_t, s_t, r_t = xin[it % 3], sq[it % 2], rstd[it % 2]
            bank = banks[it % 2]
            it += 1
            P.dma("sp", x_t[:], uT.ap()[r0:r0 + 64, tt * 512:(tt + 1) * 512])
            P.act(s_t[:], x_t[:], AF.Square)
            P.mm(bank[0:64, :], ones_bf[0:64, 0:64], s_t[:])
            P.act(r_t[:], bank[0:64, :], AF.Sqrt, bias=EPS, scale=1.0 / 64)
            P.recip(r_t[:], r_t[:])
            P.stt(dst[0:64, tt * 512:(tt + 1) * 512], x_t[:], gq[:, gi:gi + 1], r_t[:], ALU.mult, ALU.mult)
    vt = [P.sb(f"nvt{i}", [128, 280], F32) for i in range(2)]
    for i in range(NTT):
        v_t = vt[i % 2]
        P.dma("sp", v_t[:], uTM.ap()[i * 128:(i + 1) * 128, 0:280])
        for br in range(2):
            P.copy(Vaug[br][:, i, :, 0:64], v_t[:, br * 128:(br + 1) * 128].rearrange("p (g d) -> p g d", g=2))
        P.act(gts[:, i, :], v_t[:, 256:280], AF.Sigmoid)
    P.release(m1)
    if upto < 2:
        P.release(m0)
        return

    m1 = P.mark()
    kv2 = P.sb("nkv2", [128, T + 16], F32)
    w1s = P.sb("nw1s", [128, 8, 256], F32)
    w1b = P.sb("nw1b", [128, 16, 256], BF16)
    w2s = P.sb("nw2s", [128, 2, 64], F32)
    w2b = P.sb("nw2b", [128, 2, 64], BF16)
    kvpe = P.sb("nkvpe", [128, 16, 256], BF16)
    hidT = P.sb("nhid", [128, 2, 256], BF16)
    g1 = P.sb("ng1", [128, 256], F32)
    g2 = P.sb("ng2", [128, 256], F32)
    ctm = P.sb("nctm", [128, 64], F32)
    csq = P.sb("ncsq", [128, 64], F32)
    cs = P.sb("ncs", [128, 4], F32)
    NC_ = NCMP
    for kv in range(2):
        for half in range(2):
            P.dma("sp", w1s[:], cmp_w1.ap()[kv].rearrange("(j p) n -> p j n", p=128)[:, half * 8:(half + 1) * 8, :])
            P.copy(w1b[:, half * 8:(half + 1) * 8, :], w1s[:], eng="pool")
        P.dma("sp", w2s[:], cmp_w2.ap()[kv].rearrange("(j p) n -> p j n", p=128))
        P.copy(w2b[:], w2s[:], eng="pool")
        for g in range(2):
            r0 = NKV0 + kv * 128 + g * 64
            P.memset(kv2[:, T - 1:T + 16], 0.0, eng="dve")
            P.dma("sp", kv2[0:64, 0:T], uT.ap()[r0:r0 + 64, 0:T])
            P.dma("sp", kv2[64:128, 0:T - 1], uT.ap()[r0:r0 + 64, 1:T])
            kvv = kv2[:, 0:T].rearrange("p (c s) -> p c s", s=16)
            P.memset(kvpe[:], 0.0, eng="pool")
            for j in range(16):
                if 2 * j < 16:
                    src = kvv[:, 0:NC_, 2 * j]
                else:
                    src = kvv[:, 1:NC_ + 1, 2 * j - 16]
                P.ts(kvpe[:, j, 0:NC_], src, pp_ap(pp, "cmp_pe", kv * 16 + j, 1), None, ALU.add)
            for hc in range(2):
                bank = banks[2 + hc]
                for j in range(16):
                    P.mm(bank[:, 0:256], w1b[:, j, hc * 128:(hc + 1) * 128], kvpe[:, j, :], start=(j == 0), stop=(j == 15))
                P.copy(g1[:], bank[:, 0:256])
                P.tt(g2[:], g1[:], g1[:], ALU.mult)
                P.ts(g2[:], g2[:], 0.044715, 1.0, ALU.mult, ALU.add)
                P.tt(g2[:], g2[:], g1[:], ALU.mult)
                P.act(g2[:], g2[:], AF.Sigmoid, scale=1.5957691216057308)
                P.tt(hidT[:, hc, :], g2[:], g1[:], ALU.mult)
            for ci in range(NCT):
                bank = banks[4 + ci]
                for hc in range(2):
                    P.mm(bank[:, 0:64], hidT[:, hc, ci * 128:(ci + 1) * 128], w2b[:, hc, :], start=(hc == 0), stop=(hc == 1))
                if kv == 0:
                    P.copy(ctm[:], bank[:, 0:64])
                    P.tt(csq[:], ctm[:], ctm[:], ALU.mult)
                    P.red(cs[:, 0:1], csq[:], ALU.add)
                    P.act(cs[:, 1:2], cs[:, 0:1], AF.Sqrt, bias=EPS, scale=1.0 / 64)
                    P.recip(cs[:, 1:2], cs[:, 1:2])
                    P.ts(ctm[:], ctm[:], cs[:, 1:2], None, ALU.mult)
                    P.tr(banks[6][0:64, 0:128], ctm[:], ident)
                    n = min(128, NC_ - ci * 128)
                    P.ts(Kc[g][0:64, ci * 128:ci * 128 + n], banks[6][0:64, 0:n], gq[:, 1:2], None, ALU.mult)
                else:
                    P.copy(Vc[:, ci, g, 0:64], bank[:, 0:64])
    P.release(m1)
    if upto < 3:
        P.release(m0)
        return

    m1 = P.mark()
    NEGMT = [P.sb(f"nNM{g}", [64, 512], BF16) for g in range(2)]
    bcol = P.sb("nbcol", [128, 320], F32)
    cbias = P.sb("ncbias", [128, 128], F32)
    cmask = P.sb("ncmask", [128, 9, 512], F32)
    ovl = P.sb("novl", [128, 2, 64], BF16)
    selc = P.sb("nselc", [128, NTT, 64], F32)
    Eb = P.sb("nE", [64, T], BF16)
    tri = P.sb("ntri", [128, 256], BF16)
    P.dma("sp", bcol[:], dc["n_bcol"].ap())
    P.dma("sp", cbias[:], dc["n_cbias"].ap())
    P.dma("sp", cmask[:].rearrange("p a b -> p (a b)"), dc["n_cmask"].ap())
    P.dma("sp", selc[:].rearrange("p a b -> p (a b)"), dc["n_selc"].ap())
    m2 = P.mark()
    stg = P.sb("nstg2", [128, T], F32)
    P.dma("sp", stg[0:64, :], dc["n_E"].ap())
    P.copy(Eb[:], stg[0:64, :])
    P.dma("sp", stg[:, 0:256], dc["n_tri"].ap())
    P.copy(tri[:], stg[:, 0:256])
    P.dma("sp", stg[:, 256:384], dc["n_ovl"].ap())
    P.copy(ovl[:].rearrange("p a b -> p (a b)"), stg[:, 256:384])
    P.release(m2)
    PT = [P.sb(f"nPT{i}", [128, 512], BF16) for i in range(3)]
    sm = [P.sb(f"nsm{i}", [128, 512], F32) for i in range(2)]
    Osb = [P.sb(f"nO{i}", [66, 512], F32) for i in range(2)]
    Isb = [P.sb(f"nI{i}", [64, 512], F32) for i in range(2)]
    acc = P.sb("nacc", [128, 4, 512], F32)
    imp = P.sb("nimp", [128, 4, 2, 64], F32)
    sc = P.sb("nsc", [128, 16], F32)
    sco = P.sb("nsco", [128, 64], F32)
    t8 = P.sb("nt8", [128, 8], F32)
    ngm = P.sb("nngm", [128, 64], F32)
    yTt = P.sb("nyT", [128, 4, 512], BF16)
    state = {"s": 0, "p": 0, "o": 0}
    SB = [banks[0], banks[1], banks[2]]
    OB = [banks[3], banks[4]]
    IB = banks[5]
    TB = banks[6]

    def finalize(o_sb, h, br, qi, first, i_sb=None):
        g = h // 4
        for s4 in range(4):
            P.tr(TB[:, 0:66], o_sb[:, s4 * 128:(s4 + 1) * 128], ident[0:66, 0:66])
            if i_sb is not None:
                P.tr(TB[:, 128:192], i_sb[:, s4 * 128:(s4 + 1) * 128], ident[0:64, 0:64])
            P.ts(sc[:, 0:1], TB[:, 64:65], 1e-30, None, ALU.add)
            P.recip(sc[:, 1:2], sc[:, 0:1])
            P.tt(sc[:, 2:3], sc[:, 1:2], gts[:, qi * 4 + s4, br * 8 + h:br * 8 + h + 1], ALU.mult)
            dst = acc[:, s4, h * 64:(h + 1) * 64]
            if first:
                P.ts(dst, TB[:, 0:64], sc[:, 2:3], None, ALU.mult)
            else:
                P.stt(dst, TB[:, 0:64], sc[:, 2:3], dst, ALU.mult, ALU.add)
            if i_sb is not None:
                idst = imp[:, s4, g, :]
                if h % 4 == 0:
                    P.ts(idst, TB[:, 128:192], sc[:, 1:2], None, ALU.mult)
                else:
                    P.stt(idst, TB[:, 128:192], sc[:, 1:2], idst, ALU.mult, ALU.add)

    for qi in range(NT):
        q0 = qi * 512
        qs = slice(q0, q0 + 512)
        for h in range(8):
            g = h // 4
            cts = [ci for ci in range(NCT) if 512 * qi - 2048 * ci + 511 >= 31]
            ob = OB[state["o"] % 2]
            o_sb, i_sb = Osb[state["o"] % 2], Isb[state["o"] % 2]
            state["o"] += 1
            pts = []
            for ci in cts:
                sb_ = SB[state["s"] % 3]
                state["s"] += 1
                p_t = PT[state["p"] % 3]
                state["p"] += 1
                s_m = sm[ci % 2]
                P.mm(sb_[:], Kc[g][0:67, ci * 128:(ci + 1) * 128], Qaug[h][0:67, qs])
                d = 512 * qi - 2048 * ci
                mi = None
                if ci == 0 and d <= 2048:
                    mi = d // 512
                elif ci == 1:
                    mi = 5 + d // 512
                if mi is not None:
                    P.tt(s_m[:], sb_[:], cmask[:, mi, :], ALU.add)
                    src = s_m
                else:
                    src = sb_
                P.act(p_t[:], src[:], AF.Exp, bias=cbias[:, h * 16 + qi * 2 + ci:h * 16 + qi * 2 + ci + 1])
                pts.append((ci, p_t))
            for n_, (ci, p_t) in enumerate(pts):
                P.mm(ob[0:66, :], Vc[:, ci, g, :], p_t[:], start=(n_ == 0), stop=(n_ == len(pts) - 1))
            for n_, (ci, p_t) in enumerate(pts):
                P.mm(IB[0:64, :], ovl[:, ci, :], p_t[:], start=(n_ == 0), stop=(n_ == len(pts) - 1))
            P.copy(o_sb[:], ob[0:66, :], eng="act")
            P.copy(i_sb[:], IB[0:64, :], eng="act")
            finalize(o_sb, h, 0, qi, True, i_sb)
        for g in range(2):
            for s4 in range(4):
                P.tt(sco[:], imp[:, s4, g, :], selc[:, qi * 4 + s4, :], ALU.add)
                P.op("dve", lambda e, a=t8, b=sco: e.max(a[:], b[:]), [sco], [t8])
                P.ts(ngm[:], sco[:], t8[:, 7:8], NEGBIG, ALU.is_lt, ALU.mult)
                P.tr(TB[0:64, 256:384], ngm[:], ident)
                P.copy(NEGMT[g][:, s4 * 128:(s4 + 1) * 128], TB[0:64, 256:384])
        for br in range(2):
            for h in range(8):
                g = h // 4
                ob = OB[state["o"] % 2]
                o_sb = Osb[state["o"] % 2]
                state["o"] += 1
                if br == 0:
                    kts = list(range(0, 4 * qi + 4))
                else:
                    kts = list(range(max(0, 4 * qi - 4), 4 * qi + 4))
                for n_, kt in enumerate(kts):
                    k0 = kt * 128
                    dlt = q0 - k0
                    lo = max(0, -dlt)
                    hi = 512 if br == 0 else min(512, 640 - dlt)
                    sb_ = SB[state["s"] % 3]
                    state["s"] += 1
                    p_t = PT[state["p"] % 3]
                    state["p"] += 1
                    P.mm(sb_[:, lo:hi], Kaug[(br, g)][0:67, k0:k0 + 128], Qaug[h][0:67, q0 + lo:q0 + hi],
                         start=True, stop=(br == 1))
                    if br == 0:
                        P.mm(sb_[:, lo:hi], Eb[:, k0:k0 + 128], NEGMT[g][:, lo:hi], start=False, stop=True)
                    bc = h * 40 + dlt // 128 + 4
                    P.act(p_t[:, lo:hi], sb_[:, lo:hi], AF.Exp, bias=bcol[:, bc:bc + 1])
                    if dlt <= 0:
                        P.tt(p_t[:, lo:lo + 128], p_t[:, lo:lo + 128], tri[:, 0:128], ALU.mult)
                    if br == 1 and dlt >= 128:
                        P.tt(p_t[:, hi - 128:hi], p_t[:, hi - 128:hi], tri[:, 128:256], ALU.mult)
                    P.mm(ob[0:66, lo:hi], Vaug[br][:, kt, g, :], p_t[:, lo:hi], start=(n_ == 0), stop=(n_ == len(kts) - 1))
                P.copy(o_sb[:], ob[0:66, :], eng="act")
                finalize(o_sb, h, 1 + br, qi, False)
        for m in range(4):
            for s4 in range(4):
                P.tr(TB[:, s4 * 128:(s4 + 1) * 128], acc[:, s4, m * 128:(m + 1) * 128], ident)
            P.copy(yTt[:, m, :], TB[:], eng="act")
        P.dma("pool", yT.ap()[512:1024, qs].rearrange("(k p) t -> p k t", p=128), yTt[:])
    P.release(m1)
    P.release(m0)


def make_pp(inp, l):
    pp = np.zeros((128, NPP), np.float32)

    def put(name, arr):
        o, w = PP[name]
        assert arr.shape == (128, w), (name, arr.shape)
        pp[:, o:o + w] = arr

    put("g_mix", inp["norm_mix"][l].reshape(8, 128).T)
    put("g_ffn", inp["norm_ffn"][l].reshape(8, 128).T)
    put("b_gate", inp["b_gate"][l].reshape(24, 128).T)
    put("conv_a", inp["conv_a_w"][l].reshape(3, 4, 128).transpose(2, 1, 0).reshape(128, 12))
    put("gdn_conv", inp["gdn_conv_w"][l].reshape(4, 12, 128).transpose(2, 1, 0).reshape(128, 48))
    put("qk_gain", np.tile(inp["nsa_qk_gain"][l].T, (2, 1)))
    put("cmp_pe", inp["cmp_pe"][l].reshape(2, 16, 2, 64).transpose(2, 3, 0, 1).reshape(128, 32))
    put("b_rt", np.tile(np.concatenate([inp["b_router_group"][l], inp["b_router_expert"][l]])[None, :], (128, 1)))
    put("a_log", np.tile(inp["gdn_a_log"][l][None, :], (128, 1)))
    put("dt_bias", np.tile(inp["gdn_dt_bias"][l][None, :], (128, 1)))
    put("out_gain", np.tile(inp["gdn_out_gain"][l][None, :], (128, 1)))
    return pp


def make_cst():
    c = np.zeros((128, 1024), np.float32)
    c[:, 0:128] = np.eye(128, dtype=np.float32)
    i = np.arange(64)
    c[0:64, 128:192] = (i[:, None] <= i[None, :])
    c[0:64, 192:256] = np.where(i[:, None] >= i[None, :], 0.0, -1e5)
    c[0:64, 256:320] = np.where(i[None, :] >= i[:, None], 0.0, -1e5)
    c[0:64, 320:384] = 1.0 - np.eye(64)
    c[:, 384:512] = 1.0
    return c


from concourse.bass_utils import run_bass_kernel_spmd

T_SEQ = 4096
N_CORES = 8
FUSED = True

_PROG_CACHE = {}


def _get_prog(L, S):
    key = (L, S)
    if key not in _PROG_CACHE:
        P, c = build(T_SEQ, L=L, S=S)
        _PROG_CACHE[key] = P.emit()
    return _PROG_CACHE[key]


def _weights(inp, layers):
    f = lambda a: np.ascontiguousarray(np.asarray(a, dtype=np.float32))
    sl = slice(layers[0], layers[-1] + 1)
    d = {
        "pp": f(np.stack([make_pp(inp, l) for l in layers])),
        "cst": make_cst(),
        "w_in": f(inp["w_in"][sl]),
        "w_gate": f(inp["w_gate"][sl]),
        "w_branch": f(inp["w_branch"][sl]),
        "w_out": f(inp["w_out"][sl]),
        "w_rt": f(np.concatenate([inp["w_router_group"][sl], inp["w_router_expert"][sl]], axis=2)),
        "w_eg": f(inp["w_expert_gate"][sl]),
        "w_eu": f(inp["w_expert_up"][sl]),
        "w_ed": f(inp["w_expert_down"][sl]),
        "cmp_w1": f(inp["cmp_w1"][sl]),
        "cmp_w2": f(inp["cmp_w2"][sl]),
    }
    d.update(nsa_consts(T_SEQ))
    return d


def kernel(**inputs):
    inp = {k: np.asarray(v) for k, v in inputs.items()}
    x = inp["x"].astype(np.float32, copy=False)
    B = x.shape[0]
    xT = np.ascontiguousarray(x.transpose(0, 2, 1))
    cores = list(range(N_CORES))
    if FUSED:
        S = B // N_CORES
        nc = _get_prog(4, S)
        w = _weights(inp, [0, 1, 2, 3])
        in_maps = [dict(w, xT=np.ascontiguousarray(xT[ci * S:(ci + 1) * S])) for ci in cores]
        res = run_bass_kernel_spmd(nc, in_maps, core_ids=cores)
        outT = np.concatenate([np.asarray(r["yT_out"]) for r in res.results], axis=0)
    else:
        nc = _get_prog(1, 1)
        cur = xT
        for l in range(4):
            w = _weights(inp, [l])
            nxt = np.empty_like(cur)
            for half in range(B // N_CORES):
                in_maps = [dict(w, xT=np.ascontiguousarray(cur[half * N_CORES + ci][None])) for ci in cores]
                res = run_bass_kernel_spmd(nc, in_maps, core_ids=cores)
                for ci in cores:
                    nxt[half * N_CORES + ci] = np.asarray(res.results[ci]["yT_out"])[0]
            cur = nxt
        outT = cur
    return np.ascontiguousarray(outT.transpose(0, 2, 1)).astype(np.float32, copy=False)
```
